# Optimizing a Trainium2 kernel written in Bass

```python
import jax
import jax.numpy as jnp
from jax import lax
import numpy as np

D_MODEL = 1024
BATCH = 4
SEQ = 4096
DEPTH = 1
DEC_BATCH = 8
DEC_SEQ = 64
PAST_LEN = 4096

CHUNK = 64
HEAD_DIM = 64
A_HEADS = 8
B_HEADS = 8
IDX_HEADS = 4
IDX_DIM = 64
TOPK_MAX = 256
ROPE_THETA = 500000.0
ROT_DIM = HEAD_DIM // 4
Q_BLOCK = 128
N_GROUPS = 4
EXPERTS_PER_GROUP = 8
N_EXPERTS = N_GROUPS * EXPERTS_PER_GROUP
TOP_K_EXPERTS = 2
D_EXPERT = 256
N_BRANCHES = 2
RMS_EPS = 1e-6
A_WIDTH = A_HEADS * HEAD_DIM
B_WIDTH = B_HEADS * HEAD_DIM
IN_WIDTH = 3 * A_WIDTH + IDX_HEADS * IDX_DIM + IDX_DIM + IDX_HEADS + 3 * B_WIDTH + N_BRANCHES * D_MODEL

kernel_name = 'hybrid_dsa_stickbreak_hmoe_stream_step'


def _rms(x, g):
    x32 = x.astype(jnp.float32)
    y = x32 * lax.rsqrt(jnp.mean(x32 * x32, axis=-1, keepdims=True) + RMS_EPS)
    return (y * g.astype(jnp.float32)).astype(x.dtype)


def _partial_rope(x, pos):
    half = ROT_DIM // 2
    freqs = ROPE_THETA ** (-jnp.arange(0, ROT_DIM, 2, dtype=jnp.float32) / ROT_DIM)
    ang = pos.astype(jnp.float32)[:, None] * freqs[None, :]
    cos = jnp.cos(ang)[None, :, None, :]
    sin = jnp.sin(ang)[None, :, None, :]
    x32 = x.astype(jnp.float32)
    x1 = x32[..., :half]
    x2 = x32[..., half:ROT_DIM]
    out = jnp.concatenate([x1 * cos - x2 * sin, x2 * cos + x1 * sin, x32[..., ROT_DIM:]], axis=-1)
    return out.astype(x.dtype)


def _to_blocks(x, blk):
    b, t = x.shape[0], x.shape[1]
    return jnp.moveaxis(x.reshape((b, t // blk, blk) + x.shape[2:]), 1, 0)


def _from_blocks(y):
    nb, b, blk = y.shape[0], y.shape[1], y.shape[2]
    return jnp.moveaxis(y, 0, 1).reshape((b, nb * blk) + y.shape[3:])


def _dsa_attention(q, q_idx, w_idx, q_pos, k, v, k_idx, k_pos):
    s_len = k.shape[1]
    topk = min(TOPK_MAX, s_len // 4)
    blk = min(Q_BLOCK, q.shape[1])
    k_chunk = k_pos // CHUNK

    def block(args):
        qb, qib, wb, pb = args
        rel = jax.nn.relu(jnp.einsum('bqhd,bsd->bqhs', qib.astype(jnp.float32), k_idx.astype(jnp.float32)) * IDX_DIM ** -0.5)
        score = jnp.einsum('bqh,bqhs->bqs', wb.astype(jnp.float32), rel)
        admissible = k_chunk[None, :] <= (pb // CHUNK)[:, None]
        score = jnp.where(admissible[None], score, -jnp.inf)
        top_val, top_idx = lax.top_k(score, topk)
        valid = jnp.isfinite(top_val)
        kg = jax.vmap(lambda kk, ii: kk[ii])(k, top_idx)
        vg = jax.vmap(lambda vv, ii: vv[ii])(v, top_idx)
        logits = jnp.einsum('bqhd,bqkhd->bhqk', qb.astype(jnp.float32), kg.astype(jnp.float32)) * HEAD_DIM ** -0.5
        logits = jnp.where(valid[:, None], logits, -jnp.inf)
        p = jax.nn.softmax(logits, axis=-1)
        return jnp.einsum('bhqk,bqkhd->bqhd', p.astype(v.dtype), vg)

    out = lax.map(block, (_to_blocks(q, blk), _to_blocks(q_idx, blk), _to_blocks(w_idx, blk), q_pos.reshape(-1, blk)))
    return _from_blocks(out)


def _stick_breaking(q, k, v, q_pos, k_pos):
    blk = min(Q_BLOCK, q.shape[1])

    def block(args):
        qb, pb = args
        z = jnp.einsum('bqhd,bshd->bhqs', qb.astype(jnp.float32), k.astype(jnp.float32)) * HEAD_DIM ** -0.5
        causal = (k_pos[None, :] < pb[:, None])[None, None]
        log_keep = jnp.where(causal, jax.nn.log_sigmoid(-z), 0.0)
        later = lax.cumsum(log_keep, axis=3, reverse=True) - log_keep
        a = jnp.where(causal, jnp.exp(jax.nn.log_sigmoid(z) + later), 0.0)
        return jnp.einsum('bhqs,bshd->bqhd', a.astype(v.dtype), v)

    out = lax.map(block, (_to_blocks(q, blk), q_pos.reshape(-1, blk)))
    return _from_blocks(out)


def _hier_moe(h, w_rg, b_rg, w_re, b_re, w_e_gate, w_e_up, w_e_down):
    b, t, d = h.shape
    n = h.reshape(b * t, d)
    g_logits = (n @ w_rg + b_rg).astype(jnp.float32)
    g_prob = jax.nn.softmax(g_logits, axis=-1)
    g_sel = jnp.argmax(g_logits, axis=-1)
    p_group = jnp.take_along_axis(g_prob, g_sel[:, None], axis=1)
    e_logits = (n @ w_re + b_re).astype(jnp.float32).reshape(-1, N_GROUPS, EXPERTS_PER_GROUP)
    e_in_group = jnp.take_along_axis(e_logits, g_sel[:, None, None], axis=1)[:, 0]
    top_val, top_i = lax.top_k(e_in_group, TOP_K_EXPERTS)
    w_top = jax.nn.softmax(top_val, axis=-1) * p_group
    expert_id = g_sel[:, None] * EXPERTS_PER_GROUP + top_i
    combine = jnp.sum(jax.nn.one_hot(expert_id, N_EXPERTS, dtype=jnp.float32) * w_top[..., None], axis=1)
    out = jnp.zeros((b * t, d), jnp.float32)
    for e in range(N_EXPERTS):
        act = jax.nn.silu(n @ w_e_gate[e]) * (n @ w_e_up[e])
        out = out + combine[:, e:e + 1] * (act @ w_e_down[e]).astype(jnp.float32)
    return out.astype(h.dtype).reshape(b, t, d)


def _layer(x, c, past_ak, past_av, past_akidx, past_bk, past_bv, p):
    b, t, _ = x.shape
    past = past_ak.shape[1]
    pos = past + jnp.arange(t, dtype=jnp.int32)
    k_pos = jnp.arange(past + t, dtype=jnp.int32)
    mod = jax.nn.silu(c) @ p['w_ada'] + p['b_ada']
    sh1, sc1, g1, sh2, sc2, g2 = [m[:, None, :] for m in jnp.split(mod, 6, axis=-1)]
    h = _rms(x, p['norm1_g']) * (1 + sc1) + sh1
    proj = h @ p['w_in']
    widths = [A_WIDTH, A_WIDTH, A_WIDTH, IDX_HEADS * IDX_DIM, IDX_DIM, IDX_HEADS, B_WIDTH, B_WIDTH, B_WIDTH, N_BRANCHES * D_MODEL]
    offsets = np.cumsum(widths)[:-1].tolist()
    qa, ka, va, qi, ki, wi, qb, kb, vb, gates = jnp.split(proj, offsets, axis=-1)
    qa = _partial_rope(_rms(qa.reshape(b, t, A_HEADS, HEAD_DIM), p['qnorm_g']), pos)
    ka = _partial_rope(_rms(ka.reshape(b, t, A_HEADS, HEAD_DIM), p['knorm_g']), pos)
    va = va.reshape(b, t, A_HEADS, HEAD_DIM)
    qi = _partial_rope(qi.reshape(b, t, IDX_HEADS, IDX_DIM), pos)
    ki = _partial_rope(ki[:, :, None, :], pos)[:, :, 0, :]
    qb = qb.reshape(b, t, B_HEADS, HEAD_DIM)
    kb = kb.reshape(b, t, B_HEADS, HEAD_DIM)
    vb = vb.reshape(b, t, B_HEADS, HEAD_DIM)
    ka_all = jnp.concatenate([past_ak, ka], axis=1)
    va_all = jnp.concatenate([past_av, va], axis=1)
    ki_all = jnp.concatenate([past_akidx, ki], axis=1)
    kb_all = jnp.concatenate([past_bk, kb], axis=1)
    vb_all = jnp.concatenate([past_bv, vb], axis=1)
    o_a = _dsa_attention(qa, qi, wi, pos, ka_all, va_all, ki_all, k_pos)
    o_b = _stick_breaking(qb, kb_all, vb_all, pos, k_pos)
    gate_a, gate_b = jnp.split(jax.nn.sigmoid(gates), 2, axis=-1)
    merged = gate_a * (o_a.reshape(b, t, A_WIDTH) @ p['w_up_a']) + gate_b * (o_b.reshape(b, t, B_WIDTH) @ p['w_up_b'])
    y = x + g1 * (merged @ p['w_out'])
    h2 = _rms(y, p['norm2_g']) * (1 + sc2) + sh2
    out = y + g2 * _hier_moe(h2, p['w_rg'], p['b_rg'], p['w_re'], p['b_re'], p['w_e_gate'], p['w_e_up'], p['w_e_down'])
    return out, (ka, va, ki, kb, vb)


def setup_inputs(seed: int = 0) -> dict:
    key = jax.random.key(seed)
    ks = jax.random.split(key, 26)

    def nrm(k, shape, scale=1.0):
        return jax.random.normal(k, shape, jnp.float32) * scale

    return {
        'x_prompt': nrm(ks[0], (BATCH, SEQ, D_MODEL)),
        'x_sample': nrm(ks[1], (DEC_BATCH, DEC_SEQ, D_MODEL)),
        'cache_a_k': nrm(ks[2], (DEPTH, DEC_BATCH, PAST_LEN, A_HEADS, HEAD_DIM)),
        'cache_a_v': nrm(ks[3], (DEPTH, DEC_BATCH, PAST_LEN, A_HEADS, HEAD_DIM)),
        'cache_a_kidx': nrm(ks[4], (DEPTH, DEC_BATCH, PAST_LEN, IDX_DIM)),
        'cache_b_k': nrm(ks[5], (DEPTH, DEC_BATCH, PAST_LEN, B_HEADS, HEAD_DIM)),
        'cache_b_v': nrm(ks[6], (DEPTH, DEC_BATCH, PAST_LEN, B_HEADS, HEAD_DIM)),
        'c_prompt': nrm(ks[7], (BATCH, D_MODEL)),
        'c_sample': nrm(ks[8], (DEC_BATCH, D_MODEL)),
        'w_ada': nrm(ks[9], (DEPTH, D_MODEL, 6 * D_MODEL), 0.5 * D_MODEL ** -0.5),
        'b_ada': nrm(ks[10], (DEPTH, 6 * D_MODEL), 0.02),
        'norm1_g': 1.0 + nrm(ks[11], (DEPTH, D_MODEL), 0.02),
        'w_in': nrm(ks[12], (DEPTH, D_MODEL, IN_WIDTH), D_MODEL ** -0.5),
        'qnorm_g': 1.0 + nrm(ks[13], (DEPTH, HEAD_DIM), 0.02),
        'knorm_g': 1.0 + nrm(ks[14], (DEPTH, HEAD_DIM), 0.02),
        'w_up_a': nrm(ks[15], (DEPTH, A_WIDTH, D_MODEL), A_WIDTH ** -0.5),
        'w_up_b': nrm(ks[16], (DEPTH, B_WIDTH, D_MODEL), B_WIDTH ** -0.5),
        'w_out': nrm(ks[17], (DEPTH, D_MODEL, D_MODEL), D_MODEL ** -0.5),
        'norm2_g': 1.0 + nrm(ks[18], (DEPTH, D_MODEL), 0.02),
        'w_rg': nrm(ks[19], (DEPTH, D_MODEL, N_GROUPS), D_MODEL ** -0.5),
        'b_rg': nrm(ks[20], (DEPTH, N_GROUPS), 0.01),
        'w_re': nrm(ks[21], (DEPTH, D_MODEL, N_EXPERTS), D_MODEL ** -0.5),
        'b_re': nrm(ks[22], (DEPTH, N_EXPERTS), 0.01),
        'w_e_gate': nrm(ks[23], (DEPTH, N_EXPERTS, D_MODEL, D_EXPERT), D_MODEL ** -0.5),
        'w_e_up': nrm(ks[24], (DEPTH, N_EXPERTS, D_MODEL, D_EXPERT), D_MODEL ** -0.5),
        'w_e_down': nrm(ks[25], (DEPTH, N_EXPERTS, D_EXPERT, D_MODEL), D_EXPERT ** -0.5),
    }


def reference(x_prompt, x_sample, cache_a_k, cache_a_v, cache_a_kidx, cache_b_k, cache_b_v, c_prompt, c_sample,
              w_ada, b_ada, norm1_g, w_in, qnorm_g, knorm_g, w_up_a, w_up_b, w_out, norm2_g,
              w_rg, b_rg, w_re, b_re, w_e_gate, w_e_up, w_e_down):
    bp = x_prompt.shape[0]
    dt = x_prompt.dtype
    empty_heads_a = jnp.zeros((bp, 0, A_HEADS, HEAD_DIM), dt)
    empty_idx = jnp.zeros((bp, 0, IDX_DIM), dt)
    empty_heads_b = jnp.zeros((bp, 0, B_HEADS, HEAD_DIM), dt)
    y_prompt = x_prompt
    y_sample = x_sample
    rows_p = []
    rows_s = []
    for l in range(DEPTH):
        p = dict(w_ada=w_ada[l], b_ada=b_ada[l], norm1_g=norm1_g[l], w_in=w_in[l], qnorm_g=qnorm_g[l],
                 knorm_g=knorm_g[l], w_up_a=w_up_a[l], w_up_b=w_up_b[l], w_out=w_out[l], norm2_g=norm2_g[l],
                 w_rg=w_rg[l], b_rg=b_rg[l], w_re=w_re[l], b_re=b_re[l],
                 w_e_gate=w_e_gate[l], w_e_up=w_e_up[l], w_e_down=w_e_down[l])
        y_prompt, new_p = _layer(y_prompt, c_prompt, empty_heads_a, empty_heads_a, empty_idx,
                                 empty_heads_b, empty_heads_b, p)
        y_sample, new_s = _layer(y_sample, c_sample, cache_a_k[l], cache_a_v[l], cache_a_kidx[l],
                                 cache_b_k[l], cache_b_v[l], p)
        rows_p.append(new_p)
        rows_s.append(new_s)
    new_a_k_prompt = jnp.stack([r[0] for r in rows_p])
    new_a_v_prompt = jnp.stack([r[1] for r in rows_p])
    new_a_kidx_prompt = jnp.stack([r[2] for r in rows_p])
    new_b_k_prompt = jnp.stack([r[3] for r in rows_p])
    new_b_v_prompt = jnp.stack([r[4] for r in rows_p])
    new_a_k_sample = jnp.stack([r[0] for r in rows_s])
    new_a_v_sample = jnp.stack([r[1] for r in rows_s])
    new_a_kidx_sample = jnp.stack([r[2] for r in rows_s])
    new_b_k_sample = jnp.stack([r[3] for r in rows_s])
    new_b_v_sample = jnp.stack([r[4] for r in rows_s])
    return (y_prompt, y_sample,
            new_a_k_prompt, new_a_v_prompt, new_a_kidx_prompt, new_b_k_prompt, new_b_v_prompt,
            new_a_k_sample, new_a_v_sample, new_a_kidx_sample, new_b_k_sample, new_b_v_sample)
```

```python
import numpy as np
import ml_dtypes
import contextlib
import concourse.bass as bass
import concourse.mybir as mybir
from concourse.bass_utils import run_bass_kernel_spmd

F32 = mybir.dt.float32
BF16 = mybir.dt.bfloat16
AF = mybir.ActivationFunctionType
ALU = mybir.AluOpType
AX = mybir.AxisListType

D = 1024
KC = 8
NTP = 32
NOWN = 16
NTS = 33
NKS = NTS * 128
TOWN = NOWN * 128 + 64
NIT = 14
NEG = -30000.0
TPAR = ((0, 3), (1, 2))
C_QA, C_KA, C_VA, C_QI, C_KI, C_WI, C_QB, C_KB, C_VB, C_G = 0, 512, 1024, 1536, 1792, 1856, 1860, 2372, 2884, 3396
NEXP = 32


class Buf:
    __slots__ = ("name", "wdeps", "rdeps", "dsem", "dcnt", "rsem", "rcnt", "excl")

    def __init__(self, name, inherit=(), excl=False):
        self.name = name
        self.excl = excl
        self.wdeps = set()
        self.rdeps = set(inherit)
        self.dsem = None
        self.dcnt = 0
        self.rsem = None
        self.rcnt = 0


class Sched:
    ENGS = ("pe", "act", "dve", "pool", "sp")

    def __init__(self, nc):
        self.nc = nc
        self.eng = {"pe": nc.tensor, "act": nc.scalar, "dve": nc.vector, "pool": nc.gpsimd, "sp": nc.sync}
        self.sem = {e: nc.alloc_semaphore(f"q_{e}") for e in self.ENGS}
        self.cnt = {e: 0 for e in self.ENGS}
        self.seen = {e: {} for e in self.ENGS}
        self.semobj = {}
        self.out_deps = set()
        self.free_dsems = []
        self.nsem = 0

    def _wait(self, e, deps):
        eng = self.eng[e]
        seen = self.seen[e]
        best = {}
        for (sem, val) in deps:
            k = id(sem)
            self.semobj[k] = sem
            if seen.get(k, 0) >= val:
                continue
            if best.get(k, 0) < val:
                best[k] = val
        for k, val in best.items():
            eng.wait_ge(self.semobj[k], val)
            seen[k] = val

    def op(self, e, fn, reads=(), writes=()):
        own = id(self.sem[e])
        deps = set()
        for b in reads:
            for d in b.wdeps:
                if id(d[0]) == own and e == "pe":
                    continue
                deps.add(d)
            if b.excl:
                for d in b.rdeps:
                    if id(d[0]) != own:
                        deps.add(d)
        for b in writes:
            for d in b.wdeps:
                if id(d[0]) != own:
                    deps.add(d)
            for d in b.rdeps:
                if id(d[0]) != own:
                    deps.add(d)
        self._wait(e, deps)
        ins = fn(self.eng[e])
        self.cnt[e] += 1
        ins.then_inc(self.sem[e], 1)
        d = (self.sem[e], self.cnt[e])
        for b in reads:
            b.rdeps = {x for x in b.rdeps if id(x[0]) != own} | {d}
        for b in writes:
            b.wdeps = {d}
            b.rdeps = set()
        return ins

    def _newsem(self, name):
        self.nsem += 1
        return self.nc.alloc_semaphore(f"{name}_{self.nsem}")

    def dma(self, e, out_ap, in_ap, wbuf=None, rbuf=None, more=False, is_output=False):
        deps = set()
        if wbuf is not None:
            if more and wbuf.dsem is not None:
                deps |= {d for d in wbuf.wdeps if id(d[0]) != id(wbuf.dsem)}
            else:
                deps |= wbuf.wdeps
            deps |= wbuf.rdeps
        if rbuf is not None:
            deps |= rbuf.wdeps
        self._wait(e, deps)
        ins = self.eng[e].dma_start(out=out_ap, in_=in_ap)
        if wbuf is not None:
            if wbuf.dsem is None:
                wbuf.dsem = self._newsem("d_" + wbuf.name)
            wbuf.dcnt += 16
            ins.then_inc(wbuf.dsem, 16)
            keep = set()
            if more:
                keep = {d for d in wbuf.wdeps if id(d[0]) != id(wbuf.dsem)}
            wbuf.wdeps = keep | {(wbuf.dsem, wbuf.dcnt)}
            wbuf.rdeps = set()
        elif rbuf is not None:
            if rbuf.rsem is None:
                rbuf.rsem = self._newsem("r_" + rbuf.name)
            rbuf.rcnt += 16
            ins.then_inc(rbuf.rsem, 16)
            d = (rbuf.rsem, rbuf.rcnt)
            rbuf.rdeps = {x for x in rbuf.rdeps if id(x[0]) != id(rbuf.rsem)} | {d}
            if is_output:
                self.out_deps = {x for x in self.out_deps if id(x[0]) != id(rbuf.rsem)} | {d}
        return ins

    def finish(self):
        deps = set(self.out_deps)
        for e in self.ENGS:
            if self.cnt[e] > 0:
                deps.add((self.sem[e], self.cnt[e]))
        self._wait("sp", deps)


class Stop(Exception):
    pass


class Region:
    def __init__(self, arena, off, size, name, inherit):
        self.arena = arena
        self.off = off
        self.size = size
        self.name = name
        self.inherit = inherit
        self.bufs = {}
        self.b = self.buf(None)

    def buf(self, key):
        if key not in self.bufs:
            self.bufs[key] = Buf(f"{self.name}_{key}", inherit=self.inherit)
        return self.bufs[key]

    def view(self, dt, dims, byte_off=0):
        esz = 2 if dt == BF16 else 4
        n = 1
        for x in dims:
            n *= x
        o = (self.off + byte_off) // 2
        assert byte_off + n * esz <= self.size, (self.name, byte_off, n, esz, self.size)
        ap = self.arena.t[:, o:o + n * esz // 2]
        if dt != BF16:
            ap = ap.bitcast(dt)
        if len(dims) == 2:
            ap = ap.rearrange("p (a b) -> p a b", a=dims[0])
        elif len(dims) == 3:
            ap = ap.rearrange("p (a b c) -> p a b c", a=dims[0], b=dims[1])
        return ap


class Arena:
    def __init__(self, t, nbytes):
        self.t = t
        self.nbytes = nbytes
        self.free = [(0, nbytes)]
        self.retired = []
        self.peak = 0

    def alloc(self, nbytes, name):
        nbytes = (nbytes + 63) // 64 * 64
        for i, (o, s) in enumerate(self.free):
            if s >= nbytes:
                if s == nbytes:
                    self.free.pop(i)
                else:
                    self.free[i] = (o + nbytes, s - nbytes)
                inh = set()
                for (a, b, deps) in self.retired:
                    if a < o + nbytes and b > o:
                        inh |= deps
                self.peak = max(self.peak, o + nbytes)
                return Region(self, o, nbytes, name, inh)
        raise RuntimeError(f"arena full allocating {name} {nbytes}; free={self.free}")

    def release(self, r):
        deps = set()
        for b in r.bufs.values():
            deps |= b.wdeps | b.rdeps
        self.retired = [(a, b, d) for (a, b, d) in self.retired if not (a >= r.off and b <= r.off + r.size)]
        self.retired.append((r.off, r.off + r.size, deps))
        self.free.append((r.off, r.size))
        self.free.sort()
        m = []
        for o, s in self.free:
            if m and m[-1][0] + m[-1][1] == o:
                m[-1] = (m[-1][0], m[-1][1] + s)
            else:
                m.append((o, s))
        self.free = m


def build(upto=9):
    nc = bass.Bass("TRN2", target_bir_lowering=False)
    S = Sched(nc)

    def din(name, shape, dt=F32):
        return nc.dram_tensor(name, list(shape), dt, kind="ExternalInput").ap()

    def dout(name, shape, dt=F32):
        return nc.dram_tensor(name, list(shape), dt, kind="ExternalOutput").ap()

    x_p = din("x_p", [4096, D]); x_own = din("x_own", [2048, D]); x_s = din("x_s", [64, D])
    ca_k = din("ca_k", [4096, 512]); ca_v = din("ca_v", [4096, 512]); ca_ki = din("ca_ki", [4096, 64])
    cb_k = din("cb_k", [4096, 512]); cb_v = din("cb_v", [4096, 512])
    cT = din("cT", [128, 16])
    w_ada = din("w_ada", [D, 6 * D]); b_adaT = din("b_adaT", [128, 48]); b_ada = din("b_ada", [1, 6 * D])
    n1gT = din("n1gT", [128, 8]); n2gT = din("n2gT", [128, 8]); gq = din("gq", [1, 64]); gk = din("gk", [1, 64])
    w_in = din("w_in", [D, 5444]); w_up = din("w_up", [128, 8, D]); w_out = din("w_out", [D, D])
    w_r = din("w_r", [D, 36]); b_r = din("b_r", [1, 36])
    w_eg = din("w_eg", [NEXP, D, 256]); w_eu = din("w_eu", [NEXP, D, 256]); w_ed = din("w_ed", [NEXP, 256, D])
    c_identb = din("c_identb", [128, 128], BF16); c_identf = din("c_identf", [128, 128])
    c_lneg = din("c_lneg", [128, 128], BF16); c_oneg = din("c_oneg", [128, 128], BF16)
    c_sbm = din("c_sbm", [128, 8 * 128], BF16); c_sbms = din("c_sbms", [64, 128], BF16)
    c_adm = din("c_adm", [128, 2 * 512])
    c_ropep = din("c_ropep", [128, NTP * 24]); c_ropeo = din("c_ropeo", [128, NOWN * 24]); c_ropes = din("c_ropes", [128, 24])
    c_pow2 = din("c_pow2", [128, 24])

    y_own = dout("y_own", [2048, D]); y_s = dout("y_s", [64, D])
    o_nak = dout("o_nak", [4096, 512]); o_nav = dout("o_nav", [4096, 512]); o_naki = dout("o_naki", [4096, 64])
    o_nbk = dout("o_nbk", [4096, 512]); o_nbv = dout("o_nbv", [4096, 512])
    s_nak = dout("s_nak", [64, 512]); s_nav = dout("s_nav", [64, 512]); s_naki = dout("s_naki", [64, 64])
    s_nbk = dout("s_nbk", [64, 512]); s_nbv = dout("s_nbv", [64, 512])

    w_in_v = w_in.rearrange("(kc p) n -> p kc n", p=128)
    w_ada_v = w_ada.rearrange("(kc p) n -> p kc n", p=128)
    w_out_v = w_out.rearrange("(kc p) n -> p kc n", p=128)
    w_r_v = w_r.rearrange("(kc p) n -> p kc n", p=128)

    def chk(x):
        if upto <= x:
            raise Stop()

    es = contextlib.ExitStack()
    ARENA_BYTES = 207 * 1024
    arena_t = es.enter_context(nc.sbuf_tensor("arena", [128, ARENA_BYTES // 2], BF16))
    AR = Arena(arena_t, ARENA_BYTES)
    banks = [es.enter_context(nc.psum_tensor(f"bank{i}", [128, 512], F32)) for i in range(8)]
    Bbank = [Buf(f"bank{i}", excl=True) for i in range(8)]

    def bankf(i):
        return banks[i][:, :]

    def bankb(i):
        return banks[i][:, :].bitcast(BF16)

    def A(nbytes, name):
        return AR.alloc(nbytes, name)

    Rc = A(128 * 2 * 3 + 128 * 4 + 1024 * 2 + 128 * 2 + 2 * 512 * 4 + 24 * 4 + 64, "consts")
    o = 0
    identb = Rc.view(BF16, [128], o); o += 256
    lneg = Rc.view(BF16, [128], o); o += 256
    oneg = Rc.view(BF16, [128], o); o += 256
    identf = Rc.view(F32, [128], o); o += 512
    sbm = Rc.view(BF16, [4, 2, 128], o); o += 2048
    sbms = Rc.view(BF16, [128], o); o += 256
    adm = Rc.view(F32, [2, 512], o); o += 4096
    pow2 = Rc.view(F32, [24], o); o += 96
    Bc = Rc.b
    S.dma("sp", identb, c_identb[:, :], wbuf=Bc)
    S.dma("sp", lneg, c_lneg[:, :], wbuf=Bc, more=True)
    S.dma("sp", oneg, c_oneg[:, :], wbuf=Bc, more=True)
    S.dma("sp", identf, c_identf[:, :], wbuf=Bc, more=True)
    S.dma("sp", sbm, c_sbm.rearrange("p (a b c) -> p a b c", a=4, b=2), wbuf=Bc, more=True)
    S.dma("sp", sbms[0:64, :], c_sbms[:, :], wbuf=Bc, more=True)
    S.dma("sp", adm, c_adm.rearrange("p (a b) -> p a b", a=2), wbuf=Bc, more=True)
    S.dma("sp", pow2, c_pow2[:, :], wbuf=Bc, more=True)

    Rc2 = A(NTP * 24 * 4 + NOWN * 24 * 4 + 24 * 4 + 64 * 4 * 2 + 36 * 4 + 8 * 36 * 4 + 16 * 4 * 3 + 64, "consts2")
    o = 0
    ropep = Rc2.view(F32, [NTP, 24], o); o += NTP * 96
    ropeo = Rc2.view(F32, [NOWN, 24], o); o += NOWN * 96
    ropes = Rc2.view(F32, [24], o); o += 96
    gqr = Rc2.view(F32, [64], o); o += 256
    gkr = Rc2.view(F32, [64], o); o += 256
    brr = Rc2.view(F32, [36], o); o += 144
    wr_sb = Rc2.view(F32, [8, 36], o); o += 8 * 36 * 4
    n1g = Rc2.view(F32, [8], o); o += 32
    n2g = Rc2.view(F32, [8], o); o += 32
    cTs = Rc2.view(F32, [16], o); o += 64
    Bc2 = Rc2.b
    S.dma("sp", ropep, c_ropep.rearrange("p (a b) -> p a b", a=NTP), wbuf=Bc2)
    S.dma("sp", ropeo, c_ropeo.rearrange("p (a b) -> p a b", a=NOWN), wbuf=Bc2, more=True)
    S.dma("sp", ropes, c_ropes[:, :], wbuf=Bc2, more=True)
    S.dma("sp", gqr, gq[0:1, :].to_broadcast([128, 64]), wbuf=Bc2, more=True)
    S.dma("sp", gkr, gk[0:1, :].to_broadcast([128, 64]), wbuf=Bc2, more=True)
    S.dma("sp", brr, b_r[0:1, :].to_broadcast([128, 36]), wbuf=Bc2, more=True)
    S.dma("sp", wr_sb, w_r_v, wbuf=Bc2, more=True)
    S.dma("sp", n1g, n1gT[:, :], wbuf=Bc2, more=True)
    S.dma("sp", n2g, n2gT[:, :], wbuf=Bc2, more=True)
    S.dma("sp", cTs, cT[:, :], wbuf=Bc2, more=True)

    Rs = A(4096, "small")
    _so = [0]
    smallbufs = {}

    def small(name, n, dt=F32):
        esz = 2 if dt == BF16 else 4
        v = Rs.view(dt, [n], _so[0])
        _so[0] += (n * esz + 15) // 16 * 16
        smallbufs[name] = Rs.buf(name)
        return v, smallbufs[name]

    epsc, Beps = small("eps", 1)
    onef, Bonef = small("onef", 64)
    zc, Bzc = small("zc", 1)
    S.op("pool", lambda e: e.memset(epsc, 1e-6), writes=[Beps])
    S.op("pool", lambda e: e.memset(onef, 1.0), writes=[Bonef])
    S.op("pool", lambda e: e.memset(zc, 0.0), writes=[Bzc])
    ssq, Bssq = small("ssq", 1)
    lnv, Blnv = small("lnv", 1)
    rstd, Brstd = small("rstd", 1)
    ss8, Bss8 = small("ss8", 8)
    ln8, Bln8 = small("ln8", 8)
    rs8, Brs8 = small("rs8", 8)
    amax, Bamax = small("amax", 1)
    w0, Bw0 = small("w0", 1)
    Wt, BWt = small("Wt", 24)
    cc, Bcc = small("cc", 1)
    cnt, Bcnt = small("cnt", 1)
    tpp, Btpp = small("tpp", 1)
    ssum, Bssum = small("ssum", 1)
    tq, Btq = small("tq", 1)
    thr, Bthr = small("thr", 1)
    wi_sb, Bwi = small("wi", 4)
    modT, BmodT = small("modT", 96)
    A1, BA1 = small("A1", 16)
    B1, BB1 = small("B1", 16)
    A2, BA2 = small("A2", 16)
    B2, BB2 = small("B2", 16)
    tm16, Btm16 = small("tm16", 16)
    silc, Bsilc = small("silc", 16)

    GR = {}

    def phase0(do_G):
        Rslab = [A(8 * 512 * 4, f"adaslab{i}") for i in range(2)]
        Rscb = A(2 * 8 * 128 * 4, "scB"); scB = Rscb.view(F32, [2, 8, 128])
        Rbrow = [A(512 * 4, f"brow{i}") for i in range(2)]
        Rbt = A(48 * 4, "badaT"); bT = Rbt.view(F32, [48])
        S.dma("sp", bT, b_adaT[:, :], wbuf=Rbt.b)
        silv = silc.rearrange("p (k j) -> p k j", j=2)
        S.op("act", lambda e: e.activation(out=silc, in_=cTs, func=AF.Silu), reads=[Bc2], writes=[Bsilc])
        for j in range(2):
            S.op("dve", lambda e, j=j: e.tensor_copy(out=scB[:, j, :, :], in_=silv[:, :, j:j + 1].to_broadcast([128, 8, 128])),
                 reads=[Bsilc], writes=[Rscb.b])
        pm = bankf(0)
        first = True
        if do_G:
            GR["RG1"] = A(2 * 1024 * 4, "G1"); GR["RG2"] = A(2 * 1024 * 4, "G2")
            G1 = GR["RG1"].view(F32, [2, 1024]); G2 = GR["RG2"].view(F32, [2, 1024])
            RG1 = GR["RG1"]; RG2 = GR["RG2"]
        for s in range(12):
            seg = s // 2
            if do_G != (seg >= 2):
                continue
            R = Rslab[s % 2]
            sl = R.view(F32, [8, 512])
            S.dma("sp" if s % 2 == 0 else "pool", sl, w_ada_v[:, :, s * 512:(s + 1) * 512], wbuf=R.b)
            if seg in (2, 5):
                Rb = Rbrow[s % 2]
                S.dma("sp", Rb.view(F32, [512]), b_ada[0:1, s * 512:(s + 1) * 512].to_broadcast([128, 512]), wbuf=Rb.b)
                G = G1 if seg == 2 else G2
                RG = RG1 if seg == 2 else RG2
                half = s % 2
                for j in range(2):
                    bk = 1 + j
                    for k in range(8):
                        S.op("pe", lambda e, j=j, k=k, bk=bk: e.matmul(bankf(bk), scB[:, j, k, :], sl[:, k, :], start=(k == 0), stop=(k == 7)),
                             reads=[Rscb.b, R.b], writes=[Bbank[bk]])
                    S.op("dve", lambda e, j=j, bk=bk: e.tensor_tensor(out=G[:, j, half * 512:(half + 1) * 512], in0=bankf(bk), in1=Rb.view(F32, [512]), op=ALU.add),
                         reads=[Bbank[bk], Rb.b], writes=[RG.b])
            else:
                for nb in range(4):
                    blk = s * 4 + nb
                    for k in range(8):
                        S.op("pe", lambda e, nb=nb, k=k, blk=blk, first=first: e.matmul(pm[:, blk * 2:blk * 2 + 2], sl[:, k, nb * 128:(nb + 1) * 128], silv[:, k, :],
                                                                                         start=first, stop=(k == 7), skip_group_check=True),
                             reads=[R.b, Bsilc], writes=[Bbank[0]])
                        first = False
        mv = modT.rearrange("p (n j) -> p n j", j=2)
        pmv = pm[:, 0:96].rearrange("p (n j) -> p n j", j=2)
        for (a, b) in (((24, 40),) if do_G else ((0, 16),)):
            S.op("dve", lambda e, a=a, b=b: e.tensor_tensor(out=mv[:, a:b, :], in0=pmv[:, a:b, :], in1=bT[:, a:b].unsqueeze(2).to_broadcast([128, b - a, 2]), op=ALU.add),
                 reads=[Bbank[0], Rbt.b], writes=[BmodT])
        for (Ax, BAx, Bx, BBx, gT, so, sh) in (((A2, BA2, B2, BB2, n2g, 32, 24),) if do_G else ((A1, BA1, B1, BB1, n1g, 8, 0),)):
            Av = Ax.rearrange("p (k j) -> p k j", j=2)
            Bv = Bx.rearrange("p (k j) -> p k j", j=2)
            t16 = tm16.rearrange("p (k j) -> p k j", j=2)
            S.op("dve", lambda e, so=so: e.tensor_scalar(out=t16, in0=mv[:, so:so + 8, :], scalar1=1.0, scalar2=None, op0=ALU.add), reads=[BmodT], writes=[Btm16])
            S.op("dve", lambda e, Av=Av, gT=gT: e.tensor_tensor(out=Av, in0=t16, in1=gT.unsqueeze(2).to_broadcast([128, 8, 2]), op=ALU.mult), reads=[Btm16, Bc2], writes=[BAx])
            S.op("dve", lambda e, Bv=Bv, sh=sh: e.tensor_copy(out=Bv, in_=mv[:, sh:sh + 8, :]), reads=[BmodT], writes=[BBx])
        for r in Rslab + [Rscb] + Rbrow + [Rbt]:
            AR.release(r)

    try:
        phase0(False)
        A1v = A1.rearrange("p (k j) -> p k j", j=2); B1v = B1.rearrange("p (k j) -> p k j", j=2)
        A2v = A2.rearrange("p (k j) -> p k j", j=2); B2v = B2.rearrange("p (k j) -> p k j", j=2)

        Rxs = [A(1024 * 4, f"xs{i}") for i in range(2)]
        Rxn = A(1024 * 2, "xn")
        RhT = [A(8 * 128 * 2, f"hT{i}") for i in range(2)]
        Rst = {n: A(512 * 4, "st_" + n) for n in ("a", "b", "c")}
        Rtmp = A(512 * 4, "tmpf")
        Rbf = A(512 * 2, "bfc")
        Rbf2 = A(512 * 2, "bfc2")
        Rt1 = A(8 * 16 * 4, "t1")
        xn_b = Rxn.view(BF16, [1024])
        junk = Rtmp.view(BF16, [1024])
        XF = {}

        def norm_tile(xs_ap, Bx, rows, Av, Bv, j, hT_ap, BhT, tpbank, tpbank2, fp32=False, junkr=None):
            jv = junk if junkr is None else junkr.view(BF16, [1024])
            Bjk = Rtmp.b if junkr is None else junkr.b
            S.op("act", lambda e: e.activation(out=jv[:rows, :], in_=xs_ap, func=AF.Square, accum_out=ssq[:rows, :]), reads=[Bx], writes=[Bjk, Bssq])
            S.op("act", lambda e: e.activation(out=lnv[:rows, :], in_=ssq[:rows, :], func=AF.Ln, scale=1.0 / D, bias=epsc[:rows, :]), reads=[Bssq, Beps], writes=[Blnv])
            S.op("act", lambda e: e.activation(out=rstd[:rows, :], in_=lnv[:rows, :], func=AF.Exp, scale=-0.5), reads=[Blnv], writes=[Brstd])
            xn = XF["v"] if fp32 else xn_b
            Bxn = XF["b"] if fp32 else Rxn.b
            S.op("dve", lambda e: e.tensor_scalar(out=xn[:rows, :], in0=xs_ap, scalar1=rstd[:rows, :], scalar2=None, op0=ALU.mult), reads=[Bx, Brstd], writes=[Bxn])
            def tpv(k):
                bk = tpbank if k < 4 else tpbank2
                if fp32:
                    return bk, bankf(bk)[:, (k % 4) * 128:(k % 4) * 128 + rows]
                return bk, bankb(bk)[:, (k % 4) * 128:(k % 4) * 128 + rows]
            idn = identf if fp32 else identb
            for k in range(8):
                bk, tp = tpv(k)
                S.op("pe", lambda e, tp=tp, k=k: e.transpose(tp, xn[:rows, k * 128:(k + 1) * 128], idn[:rows, :rows]), reads=[Bxn, Bc], writes=[Bbank[bk]])
            for kk in range(8):
                k = (kk // 2) + 4 * (kk % 2)
                bk, tp = tpv(k)
                if k < 4:
                    S.op("dve", lambda e, tp=tp, k=k: e.tensor_scalar(out=hT_ap[:, k, :rows], in0=tp, scalar1=Av[:, k, j:j + 1], scalar2=Bv[:, k, j:j + 1], op0=ALU.mult, op1=ALU.add),
                         reads=[Bbank[bk], BA1, BB1, BA2, BB2], writes=[BhT.buf(k)])
                else:
                    S.op("act", lambda e, tp=tp, k=k: e.activation(out=hT_ap[:, k, :rows], in_=tp, func=AF.Identity, scale=Av[:, k, j:j + 1], bias=Bv[:, k, j:j + 1]),
                         reads=[Bbank[bk], BA1, BB1, BA2, BB2], writes=[BhT.buf(k)])

        def proj(hT_ap, BhT, rows, slab, Bslab, c0, n, bk):
            for k in range(8):
                S.op("pe", lambda e, k=k: e.matmul(bankf(bk)[:rows, 0:n], hT_ap[:, k, :rows], slab[:, k, c0:c0 + n], start=(k == 0), stop=(k == 7)),
                     reads=[BhT.buf(k), Bslab], writes=[Bbank[bk]])

        def headnorm(st, Bst, rows, grow, nheads=8):
            tmp = Rtmp.view(F32, [512])
            n = nheads * 64
            S.op("dve", lambda e: e.tensor_tensor(out=tmp[:rows, :n], in0=st[:rows, :n], in1=st[:rows, :n], op=ALU.mult), reads=[Bst], writes=[Rtmp.b])
            S.op("dve", lambda e: e.reduce_sum(out=ss8[:rows, :nheads], in_=tmp[:rows, :n].rearrange("p (h d) -> p h d", d=64), axis=AX.X), reads=[Rtmp.b], writes=[Bss8])
            S.op("act", lambda e: e.activation(out=ln8[:rows, :nheads], in_=ss8[:rows, :nheads], func=AF.Ln, scale=1.0 / 64, bias=epsc[:rows, :]), reads=[Bss8, Beps], writes=[Bln8])
            S.op("act", lambda e: e.activation(out=rs8[:rows, :nheads], in_=ln8[:rows, :nheads], func=AF.Exp, scale=-0.5), reads=[Bln8], writes=[Brs8])
            sv = st[:rows, :n].rearrange("p (h d) -> p h d", d=64)
            S.op("dve", lambda e: e.tensor_tensor(out=sv, in0=sv, in1=rs8[:rows, :nheads].unsqueeze(2).to_broadcast([rows, nheads, 64]), op=ALU.mult), reads=[Bst, Brs8], writes=[Bst])
            S.op("dve", lambda e: e.tensor_tensor(out=sv, in0=sv, in1=grow[:rows, :].unsqueeze(1).to_broadcast([rows, nheads, 64]), op=ALU.mult), reads=[Bst, Bc2], writes=[Bst])

        def rope(st, Bst, rows, nheads, rt):
            sv = st[:rows, :nheads * 64].rearrange("p (h d) -> p h d", d=64)
            t1 = Rt1.view(F32, [8, 16])
            sinb = rt[:rows, 16:24].unsqueeze(1).to_broadcast([rows, nheads, 8])
            cosb = rt[:rows, 0:16].unsqueeze(1).to_broadcast([rows, nheads, 16])
            S.op("pool", lambda e: e.tensor_tensor(out=t1[:rows, :nheads, 0:8], in0=sv[:, :, 8:16], in1=sinb, op=ALU.mult), reads=[Bst, Bc2], writes=[Rt1.b])
            S.op("pool", lambda e: e.tensor_tensor(out=t1[:rows, :nheads, 8:16], in0=sv[:, :, 0:8], in1=sinb, op=ALU.mult), reads=[Bst, Bc2], writes=[Rt1.b])
            S.op("pool", lambda e: e.tensor_tensor(out=sv[:, :, 0:16], in0=sv[:, :, 0:16], in1=cosb, op=ALU.mult), reads=[Bst, Bc2, Rt1.b], writes=[Bst])
            S.op("pool", lambda e: e.tensor_tensor(out=sv[:, :, 0:8], in0=sv[:, :, 0:8], in1=t1[:rows, :nheads, 0:8], op=ALU.subtract), reads=[Bst, Rt1.b], writes=[Bst])
            S.op("pool", lambda e: e.tensor_tensor(out=sv[:, :, 8:16], in0=sv[:, :, 8:16], in1=t1[:rows, :nheads, 8:16], op=ALU.add), reads=[Bst, Rt1.b], writes=[Bst])

        def to_T(src_bf, Bsrc, rows, npair, dstT, BdstT, col0, bk, eng="act", zpad=False):
            tp = bankb(bk)
            for p_ in range(npair):
                S.op("pe", lambda e, p_=p_: e.transpose(tp[:, p_ * 128:p_ * 128 + rows], src_bf[:rows, p_ * 128:(p_ + 1) * 128], identb[:rows, :rows]), reads=[Bsrc, Bc], writes=[Bbank[bk]])
            src = tp[:, 0:npair * 128].rearrange("p (a b) -> p a b", a=npair)[:, :, :rows]
            if zpad:
                for hh in range(2):
                    lo = hh * 64
                    if eng == "act":
                        S.op("act", lambda e: e.activation(out=dstT[lo:lo + 64, :, hh, col0:col0 + rows], in_=src[lo:lo + 64], func=AF.Copy), reads=[Bbank[bk]], writes=[BdstT])
                    else:
                        S.op("dve", lambda e: e.tensor_copy(out=dstT[lo:lo + 64, :, hh, col0:col0 + rows], in_=src[lo:lo + 64]), reads=[Bbank[bk]], writes=[BdstT])
                return
            if eng == "act":
                S.op("act", lambda e: e.activation(out=dstT[:, :, col0:col0 + rows], in_=src, func=AF.Copy), reads=[Bbank[bk]], writes=[BdstT])
            else:
                S.op("dve", lambda e: e.tensor_copy(out=dstT[:, :, col0:col0 + rows], in_=src), reads=[Bbank[bk]], writes=[BdstT])

        RoT = A(8 * TOWN * 2, "oT"); oT = RoT.view(BF16, [8, TOWN])

        def mixer_A(group):
            prompt = group == "p"
            NT = NTP if prompt else NTS
            NKT = NT * 128
            jmod = 0 if prompt else 1
            RkaT = A(4 * NKT * 2, "kaT"); kaT = RkaT.view(BF16, [4, NKT])
            RkiT = A(NKT * 2, "kiT"); kiT = RkiT.view(BF16, [NKT])
            Rva = A(NT * 520 * 2, "va"); va = Rva.view(BF16, [NT, 8, 65])
            S.op("pool", lambda e: e.memset(va[:, :, :, 64:65], 1.0), writes=[Rva.b])
            if not prompt:
                S.op("pool", lambda e: e.memset(va[64:128, NT - 1, :, :], 0.0), writes=[Rva.b])
                S.op("pool", lambda e: e.memset(kaT[:, :, NKT - 64:NKT], 0.0), writes=[RkaT.b])
                S.op("pool", lambda e: e.memset(kiT[:, NKT - 64:NKT], 0.0), writes=[RkiT.b])
            Rslab = A(8 * 1088 * 2, "KAslab"); slab = Rslab.view(BF16, [8, 1088])
            S.dma("pool", slab[:, :, 0:1024], w_in_v[:, :, C_KA:C_KA + 1024], wbuf=Rslab.b)
            S.dma("pool", slab[:, :, 1024:1088], w_in_v[:, :, C_KI:C_KI + 64], wbuf=Rslab.b, more=True)
            Rst2 = {n: A(512 * 4, "st2_" + n) for n in ("a", "b", "c", "d", "e", "f")}
            Rjk = A(1024 * 2, "kjunk")
            STK = [(Rst["a"], Rst["b"], Rst["c"]), (Rst2["a"], Rst2["b"], Rst2["c"]), (Rst2["d"], Rst2["e"], Rst2["f"])]
            kbf = Rbf.view(BF16, [512]); ibf = Rbf2.view(BF16, [512])

            def kfront(t):
                Ra, Rb_, Rc_ = STK[t % 3]
                st_k = Ra.view(F32, [512]); st_v = Rb_.view(F32, [512]); st_i = Rc_.view(F32, [512])
                Bsk, Bsv, Bsi = Ra.b, Rb_.b, Rc_.b
                rows = 128 if (prompt or t < NT - 1) else 64
                from_cache = (not prompt) and t < NT - 1
                if from_cache:
                    S.dma("sp", st_k, ca_k[t * 128:(t + 1) * 128, :], wbuf=Bsk)
                    S.dma("sp", st_v, ca_v[t * 128:(t + 1) * 128, :], wbuf=Bsv)
                    S.dma("sp", st_i[:, 0:64], ca_ki[t * 128:(t + 1) * 128, :], wbuf=Bsi)
                else:
                    Rx = Rxs[t % 2]; xs = Rx.view(F32, [1024])
                    src = x_p[t * 128:(t + 1) * 128, :] if prompt else x_s[:, :]
                    S.dma("sp", xs[:rows, :], src, wbuf=Rx.b)
                    RH = RhT[t % 2]; hT = RH.view(BF16, [8, 128])
                    norm_tile(xs[:rows, :], Rx.b, rows, A1v, B1v, jmod, hT, RH, 0, 6, junkr=Rjk)
                    proj(hT, RH, rows, slab, Rslab.b, 0, 512, 1)
                    proj(hT, RH, rows, slab, Rslab.b, 512, 512, 2)
                    proj(hT, RH, rows, slab, Rslab.b, 1024, 64, 3)
                    S.op("act", lambda e: e.activation(out=st_k[:rows, :], in_=bankf(1)[:rows, :], func=AF.Copy), reads=[Bbank[1]], writes=[Bsk])
                    S.op("dve", lambda e: e.tensor_copy(out=st_v[:rows, :], in_=bankf(2)[:rows, :]), reads=[Bbank[2]], writes=[Bsv])
                    S.op("dve", lambda e: e.tensor_copy(out=st_i[:rows, 0:64], in_=bankf(3)[:rows, 0:64]), reads=[Bbank[3]], writes=[Bsi])

            def kback1(t):
                Ra, Rb_, Rc_ = STK[t % 3]
                st_k = Ra.view(F32, [512]); st_v = Rb_.view(F32, [512]); st_i = Rc_.view(F32, [512])
                Bsk, Bsv, Bsi = Ra.b, Rb_.b, Rc_.b
                rows = 128 if (prompt or t < NT - 1) else 64
                from_cache = (not prompt) and t < NT - 1
                if not from_cache:
                    rt = ropep[:, t, :] if prompt else ropes
                    headnorm(st_k, Bsk, rows, gkr)
                    rope(st_k, Bsk, rows, 8, rt)
                    rope(st_i, Bsi, rows, 1, rt)
                    if prompt:
                        S.dma("pool", o_nak[t * 128:(t + 1) * 128, :], st_k, rbuf=Bsk, is_output=True)
                        S.dma("pool", o_nav[t * 128:(t + 1) * 128, :], st_v, rbuf=Bsv, is_output=True)
                        S.dma("pool", o_naki[t * 128:(t + 1) * 128, :], st_i[:, 0:64], rbuf=Bsi, is_output=True)
                    else:
                        S.dma("pool", s_nak[:, :], st_k[:64, :], rbuf=Bsk, is_output=True)
                        S.dma("pool", s_nav[:, :], st_v[:64, :], rbuf=Bsv, is_output=True)
                        S.dma("pool", s_naki[:, :], st_i[:64, 0:64], rbuf=Bsi, is_output=True)

            def kback2(t):
                Ra, Rb_, Rc_ = STK[t % 3]
                st_k = Ra.view(F32, [512]); st_v = Rb_.view(F32, [512]); st_i = Rc_.view(F32, [512])
                Bsk, Bsv, Bsi = Ra.b, Rb_.b, Rc_.b
                rows = 128 if (prompt or t < NT - 1) else 64
                S.op("act", lambda e: e.activation(out=kbf[:rows, :], in_=st_k[:rows, :], func=AF.Copy), reads=[Bsk], writes=[Rbf.b])
                to_T(kbf, Rbf.b, rows, 4, kaT, RkaT.b, t * 128, 4, eng="act")
                S.op("dve", lambda e: e.tensor_copy(out=va[:rows, t, :, 0:64], in_=st_v[:rows, :].rearrange("p (h d) -> p h d", d=64)), reads=[Bsv], writes=[Rva.b])
                S.op("dve", lambda e: e.tensor_copy(out=ibf[:rows, 0:128].rearrange("p (a d) -> p a d", a=2), in_=st_i[:rows, 0:64].unsqueeze(1).to_broadcast([rows, 2, 64])), reads=[Bsi], writes=[Rbf2.b])
                tp = bankb(5)
                S.op("pe", lambda e: e.transpose(tp[:, 0:rows], ibf[:rows, 0:128], identb[:rows, :rows]), reads=[Rbf2.b, Bc], writes=[Bbank[5]])
                S.op("dve", lambda e: e.tensor_copy(out=kiT[:, t * 128:t * 128 + rows], in_=tp[:, 0:rows]), reads=[Bbank[5]], writes=[RkiT.b])

            for step in range(NT + 2):
                if step < NT:
                    kfront(step)
                if 1 <= step <= NT:
                    kback1(step - 1)
                if step >= 2:
                    kback2(step - 2)
            for r_ in list(Rst2.values()) + [Rjk]:
                AR.release(r_)
            AR.release(Rslab)
            chk(0.5 if prompt else 1.5)

            Rqs = A(8 * 772 * 2, "QAslab"); qs = Rqs.view(BF16, [8, 772])
            S.dma("pool", qs[:, :, 0:512], w_in_v[:, :, C_QA:C_QA + 512], wbuf=Rqs.b)
            S.dma("pool", qs[:, :, 512:768], w_in_v[:, :, C_QI:C_QI + 256], wbuf=Rqs.b, more=True)
            S.dma("pool", qs[:, :, 768:772], w_in_v[:, :, C_WI:C_WI + 4], wbuf=Rqs.b, more=True)
            Rsc = A(NKT * 4, "score"); score = Rsc.view(F32, [NKT])
            nb2 = 2 if prompt else 1
            Rmb = [A(NKT * 2, f"mb{i}") for i in range(nb2)] * (3 - nb2)
            Rrt = [A(512 * 4, f"rtmp{i}") for i in range(2)]
            RPt = [A(512 * 2, f"Pt{i}") for i in range(2)]
            RqaT = [A(8 * 128 * 2, f"qaT{i}") for i in range(2)]
            RqiT = [A(4 * 128 * 2, f"qiT{i}") for i in range(2)]
            for r_ in RqaT + RqiT:
                S.op("pool", lambda e, r_=r_: e.memset(r_.view(BF16, [r_.size // 2]), 0.0), writes=[r_.b])
            Rbcs = A(512 * 4, "bcs"); bcs = Rbcs.view(F32, [512])
            rden = bcs
            Brd = Rbcs.buf("rd"); Bbcs = Rbcs.buf("bc")
            st_q = Rst["a"].view(F32, [512]); st_qi = Rst["b"].view(F32, [512])
            Bsq, Bsqi = Rst["a"].b, Rst["b"].b
            qbf = Rbf.view(BF16, [512]); qibf = Rbf2.view(BF16, [512])
            ntiles = NOWN if prompt else 1
            ucount = [0]

            def qside(m):
                rows = 128 if prompt else 64
                Rx = Rxs[m % 2]; xs = Rx.view(F32, [1024])
                src = x_own[m * 128:(m + 1) * 128, :] if prompt else x_s[:, :]
                S.dma("sp", xs[:rows, :], src, wbuf=Rx.b)
                RH = RhT[m % 2]; hT = RH.view(BF16, [8, 128])
                norm_tile(xs[:rows, :], Rx.b, rows, A1v, B1v, jmod, hT, RH, 0, 7)
                proj(hT, RH, rows, qs, Rqs.b, 0, 512, 1)
                proj(hT, RH, rows, qs, Rqs.b, 512, 260, 0)
                S.op("act", lambda e: e.activation(out=st_q[:rows, :], in_=bankf(1)[:rows, :], func=AF.Copy), reads=[Bbank[1]], writes=[Bsq])
                S.op("dve", lambda e: e.tensor_copy(out=st_qi[:rows, 0:260], in_=bankf(0)[:rows, 0:260]), reads=[Bbank[0]], writes=[Bsqi])
                rt = ropeo[:, m, :] if prompt else ropes
                headnorm(st_q, Bsq, rows, gqr)
                rope(st_q, Bsq, rows, 8, rt)
                rope(st_qi, Bsqi, rows, 4, rt)
                S.op("pool", lambda e: e.tensor_scalar(out=qbf[:rows, :], in0=st_q[:rows, :], scalar1=0.125, scalar2=None, op0=ALU.mult), reads=[Bsq], writes=[Rbf.b])
                S.op("pool", lambda e: e.tensor_scalar(out=qibf[:rows, 0:256], in0=st_qi[:rows, 0:256], scalar1=0.125, scalar2=None, op0=ALU.mult), reads=[Bsqi], writes=[Rbf2.b])
                S.op("dve", lambda e: e.tensor_copy(out=wi_sb[:rows, :], in_=st_qi[:rows, 256:260]), reads=[Bsqi], writes=[Bwi])
                Rq = RqaT[m % 2]; Ri = RqiT[m % 2]
                to_T(qbf, Rbf.b, rows, 4, Rq.view(BF16, [4, 2, 128]), Rq.b, 0, 1, eng="act", zpad=True)
                to_T(qibf, Rbf2.b, rows, 2, Ri.view(BF16, [2, 2, 128]), Ri.b, 0, 0, eng="dve", zpad=True)

            def scores(m):
                rows = 128 if prompt else 64
                j = m // 2
                NK = 512 * (j + 1) if prompt else NKT
                qiT = RqiT[m % 2].view(BF16, [2, 2, 128]); BqiT = RqiT[m % 2].b
                mb = Rmb[m % 2].view(BF16, [NKT]); Bmb = Rmb[m % 2].b
                nch = (NK + 511) // 512
                for c in range(nch):
                    n = min(512, NK - c * 512)
                    for h in range(4):
                        bk = h % 2
                        S.op("pe", lambda e, h=h, c=c, n=n, bk=bk: e.matmul(bankf(bk)[:rows, 0:n], qiT[:, h // 2, h % 2, :rows], kiT[:, c * 512:c * 512 + n], start=True, stop=True),
                             reads=[BqiT, RkiT.b], writes=[Bbank[bk]])
                        sc = score[:rows, c * 512:c * 512 + n]
                        Rr = Rrt[h % 2]; rt_ = Rr.view(F32, [512])
                        S.op("act", lambda e, n=n, bk=bk, rt_=rt_: e.activation(out=rt_[:rows, 0:n], in_=bankf(bk)[:rows, 0:n], func=AF.Relu), reads=[Bbank[bk]], writes=[Rr.b])
                        if h == 0:
                            S.op("pool", lambda e, sc=sc, n=n, rt_=rt_: e.tensor_scalar(out=sc, in0=rt_[:rows, 0:n], scalar1=wi_sb[:rows, 0:1], scalar2=None, op0=ALU.mult),
                                 reads=[Rr.b, Bwi], writes=[Rsc.b])
                        else:
                            S.op("pool", lambda e, n=n, h=h, rt_=rt_: e.tensor_scalar(out=rt_[:rows, 0:n], in0=rt_[:rows, 0:n], scalar1=wi_sb[:rows, h:h + 1], scalar2=None, op0=ALU.mult),
                                 reads=[Rr.b, Bwi], writes=[Rr.b])
                            S.op("pool", lambda e, sc=sc, n=n, rt_=rt_: e.tensor_tensor(out=sc, in0=sc, in1=rt_[:rows, 0:n], op=ALU.add),
                                 reads=[Rr.b, Rsc.b], writes=[Rsc.b])
                S.op("dve", lambda e: e.reduce_max(out=amax[:rows, :], in_=score[:rows, 0:NK], axis=AX.X, apply_absolute_value=True), reads=[Rsc.b], writes=[Bamax])
                S.op("dve", lambda e: e.tensor_scalar(out=w0[:rows, :], in0=amax[:rows, :], scalar1=1.0, scalar2=None, op0=ALU.add), reads=[Bamax], writes=[Bw0])
                S.op("dve", lambda e: e.tensor_scalar(out=Wt[:rows, :], in0=pow2[:rows, :], scalar1=w0[:rows, :], scalar2=None, op0=ALU.mult), reads=[Bw0, Bc], writes=[BWt])
                if prompt:
                    S.op("dve", lambda e: e.tensor_tensor(out=score[:rows, NK - 512:NK], in0=score[:rows, NK - 512:NK], in1=adm[:rows, m % 2, :], op=ALU.add), reads=[Rsc.b, Bc], writes=[Rsc.b])
                else:
                    S.op("dve", lambda e: e.memset(score[:rows, NK - 64:NK], -1e30), writes=[Rsc.b])
                S.op("dve", lambda e: e.memset(cc[:rows, :], 0.0), writes=[Bcc])

            def bis_params(m):
                rows = 128 if prompt else 64
                NK = 512 * (m // 2 + 1) if prompt else NKT
                a = max(64, int(0.42 * NK) // 64 * 64)
                return rows, NK, a

            def bis_iter(m, it):
                rows, NK, a = bis_params(m)
                mb = Rmb[m % 2].view(BF16, [NKT])
                Blo = Rmb[m % 2].buf("lo"); Bhi = Rmb[m % 2].buf("hi")
                S.op("dve", lambda e: e.tensor_scalar(out=mb[:rows, 0:NK], in0=score[:rows, 0:NK], scalar1=cc[:rows, :], scalar2=None, op0=ALU.is_ge, op1=ALU.add, accum_out=cnt[:rows, :]),
                     reads=[Rsc.b, Bcc], writes=[Blo, Bhi, Bcnt])
                S.op("dve", lambda e: e.tensor_scalar(out=tpp[:rows, :], in0=cnt[:rows, :], scalar1=255.5, scalar2=0.5, op0=ALU.is_ge, op1=ALU.subtract), reads=[Bcnt], writes=[Btpp])
                S.op("dve", lambda e: e.scalar_tensor_tensor(out=cc[:rows, :], in0=tpp[:rows, :], scalar=Wt[:rows, it:it + 1], in1=cc[:rows, :], op0=ALU.mult, op1=ALU.add),
                     reads=[Btpp, BWt, Bcc], writes=[Bcc])

            def bis_final(m):
                rows, NK, a = bis_params(m)
                mb = Rmb[m % 2].view(BF16, [NKT])
                Blo = Rmb[m % 2].buf("lo"); Bhi = Rmb[m % 2].buf("hi")
                S.op("dve", lambda e: e.tensor_tensor(out=thr[:rows, :], in0=cc[:rows, :], in1=Wt[:rows, NIT:NIT + 1], op=ALU.subtract), reads=[Bcc, BWt], writes=[Bthr])
                S.op("dve", lambda e: e.tensor_scalar(out=mb[:rows, 0:NK], in0=score[:rows, 0:NK], scalar1=thr[:rows, :], scalar2=NEG, op0=ALU.is_lt, op1=ALU.mult),
                     reads=[Rsc.b, Bthr], writes=[Blo, Bhi])

            def attn(m, tick=None):
                rows = 128 if prompt else 64
                j = m // 2
                NKB = 4 * (j + 1) if prompt else NT
                HG = 512 // rows
                qaT = RqaT[m % 2].view(BF16, [4, 2, 128]); BqaT = RqaT[m % 2].b
                mb = Rmb[m % 2].view(BF16, [NKT])
                Blo = Rmb[m % 2].buf("lo"); Bhi = Rmb[m % 2].buf("hi")
                tok0 = m * 128 if prompt else NOWN * 128
                for g in range(8 // HG):
                    bo = 4 + (ucount[0] % 2)
                    ucount[0] += 1
                    Ov = bankf(bo)[:, 0:HG * rows].rearrange("p (a b) -> p a b", a=HG)
                    units = list(range(NKB))

                    def s_stage(kb, u):
                        bs = 2 + (u % 2)
                        for hs in range(HG):
                            h = g * HG + hs
                            pr, hh = h // 2, h % 2
                            S.op("pe", lambda e, hs=hs, pr=pr, hh=hh, kb=kb, bs=bs: e.matmul(bankf(bs)[:, hs * rows:(hs + 1) * rows], kaT[:, pr, kb * 128:(kb + 1) * 128], qaT[:, pr, hh, :rows], start=(hs == 0), stop=False, skip_group_check=True),
                                 reads=[RkaT.b, BqaT], writes=[Bbank[bs]])
                        for hs in range(HG):
                            S.op("pe", lambda e, hs=hs, kb=kb, bs=bs: e.matmul(bankf(bs)[:, hs * rows:(hs + 1) * rows], mb[:rows, kb * 128:(kb + 1) * 128], identb[:rows, :rows],
                                                                             start=False, stop=(hs == HG - 1), skip_group_check=True),
                                 reads=[Blo, Bhi, Bc], writes=[Bbank[bs]])

                    def p_stage(kb, u):
                        bs = 2 + (u % 2)
                        RP = RPt[u % 2]; Pt = RP.view(BF16, [512])
                        S.op("act", lambda e: e.activation(out=Pt[:, 0:HG * rows], in_=bankf(bs)[:, 0:HG * rows], func=AF.Exp), reads=[Bbank[bs]], writes=[RP.b])
                        for hs in range(HG):
                            h = g * HG + hs
                            S.op("pe", lambda e, hs=hs, h=h, kb=kb: e.matmul(bankf(bo)[0:65, hs * rows:(hs + 1) * rows], va[:, kb, h, :], Pt[:, hs * rows:(hs + 1) * rows],
                                                                           start=(kb == 0 and hs == 0), stop=(kb == NKB - 1), skip_group_check=True),
                                 reads=[Rva.b, RP.b], writes=[Bbank[bo]])

                    for u in range(NKB + 1):
                        if u < NKB:
                            s_stage(units[u], u)
                            chk(0.73)
                        if u >= 1:
                            p_stage(units[u - 1], u - 1)
                            chk(0.74)
                            if tick is not None:
                                tick()
                    chk(0.75)
                    n = HG * rows
                    S.op("act", lambda e: e.activation(out=rden[64:65, 0:n], in_=bankf(bo)[64:65, 0:n], func=AF.Ln), reads=[Bbank[bo]], writes=[Brd])
                    S.op("act", lambda e: e.activation(out=rden[64:65, 0:n], in_=rden[64:65, 0:n], func=AF.Exp, scale=-1.0), reads=[Brd], writes=[Brd])
                    S.op("pe", lambda e: e.matmul(bankf(6)[0:64, 0:n], onef[64:65, 0:64], rden[64:65, 0:n], start=True, stop=True), reads=[Bonef, Brd], writes=[Bbank[6]])
                    S.op("act", lambda e: e.activation(out=bcs[0:64, 0:n], in_=bankf(6)[0:64, 0:n], func=AF.Copy), reads=[Bbank[6]], writes=[Bbcs])
                    S.op("dve", lambda e, g=g: e.tensor_tensor(out=oT[0:64, g * HG:(g + 1) * HG, tok0:tok0 + rows], in0=Ov[0:64, :, :],
                                                              in1=bcs[0:64, 0:n].rearrange("p (a b) -> p a b", a=HG), op=ALU.mult),
                         reads=[Bbank[bo], Bbcs], writes=[RoT.buf("a")])

            qside(0)
            chk(0.6 if prompt else 1.6)
            scores(0)
            for it in range(NIT):
                bis_iter(0, it)
            bis_final(0)
            chk(0.7 if prompt else 1.7)
            for m in range(ntiles):
                pend = []
                if m + 1 < ntiles:
                    qside(m + 1)
                    scores(m + 1)
                    pend = list(range(NIT))
                nun = (8 // (512 // (128 if prompt else 64))) * (4 * (m // 2 + 1) if prompt else NT)
                state = {"done": 0, "units": 0}

                def tick(m=m, pend=pend, state=state, nun=nun):
                    state["units"] += 1
                    target = min(len(pend), (state["units"] * len(pend) + nun - 1) // nun)
                    while state["done"] < target:
                        bis_iter(m + 1, pend[state["done"]])
                        state["done"] += 1
                attn(m, tick)
                while state["done"] < len(pend):
                    bis_iter(m + 1, pend[state["done"]])
                    state["done"] += 1
                if m + 1 < ntiles:
                    bis_final(m + 1)
                chk(0.8 if prompt else 1.8)
            for r in [Rqs, Rsc] + Rmb[:nb2] + Rrt + RPt + RqaT + RqiT + [Rbcs, RkaT, RkiT, Rva]:
                AR.release(r)

        def mixer_B(group):
            prompt = group == "p"
            NT = NTP if prompt else NTS
            NKT = NT * 128
            jmod = 0 if prompt else 1
            RkbT = A(4 * NKT * 2, "kbT"); kbT = RkbT.view(BF16, [4, NKT])
            Rvb = A(NT * 512 * 2, "vb"); vb = Rvb.view(BF16, [NT, 512])
            if not prompt:
                S.op("pool", lambda e: e.memset(vb[64:128, NT - 1, :], 0.0), writes=[Rvb.b])
                S.op("pool", lambda e: e.memset(kbT[:, :, NKT - 64:NKT], 0.0), writes=[RkbT.b])
            Rslab = A(8 * 1024 * 2, "KBslab"); slab = Rslab.view(BF16, [8, 1024])
            S.dma("pool", slab, w_in_v[:, :, C_KB:C_KB + 1024], wbuf=Rslab.b)
            Rst2 = {n: A(512 * 4, "st2b_" + n) for n in ("a", "b", "c", "d")}
            Rjk = A(1024 * 2, "kjunkb")
            STK = [(Rst["a"], Rst["b"]), (Rst2["a"], Rst2["b"]), (Rst2["c"], Rst2["d"])]
            kbf = Rbf.view(BF16, [512])

            def kfront(t):
                Ra, Rb_ = STK[t % 3]
                st_k = Ra.view(F32, [512]); st_v = Rb_.view(F32, [512])
                Bsk, Bsv = Ra.b, Rb_.b
                rows = 128 if (prompt or t < NT - 1) else 64
                from_cache = (not prompt) and t < NT - 1
                if from_cache:
                    S.dma("sp", st_k, cb_k[t * 128:(t + 1) * 128, :], wbuf=Bsk)
                    S.dma("sp", st_v, cb_v[t * 128:(t + 1) * 128, :], wbuf=Bsv)
                else:
                    Rx = Rxs[t % 2]; xs = Rx.view(F32, [1024])
                    src = x_p[t * 128:(t + 1) * 128, :] if prompt else x_s[:, :]
                    S.dma("sp", xs[:rows, :], src, wbuf=Rx.b)
                    RH = RhT[t % 2]; hT = RH.view(BF16, [8, 128])
                    norm_tile(xs[:rows, :], Rx.b, rows, A1v, B1v, jmod, hT, RH, 0, 6, junkr=Rjk)
                    proj(hT, RH, rows, slab, Rslab.b, 0, 512, 1)
                    proj(hT, RH, rows, slab, Rslab.b, 512, 512, 2)
                    S.op("act", lambda e: e.activation(out=st_k[:rows, :], in_=bankf(1)[:rows, :], func=AF.Copy), reads=[Bbank[1]], writes=[Bsk])
                    S.op("dve", lambda e: e.tensor_copy(out=st_v[:rows, :], in_=bankf(2)[:rows, :]), reads=[Bbank[2]], writes=[Bsv])

            def kback(t):
                Ra, Rb_ = STK[t % 3]
                st_k = Ra.view(F32, [512]); st_v = Rb_.view(F32, [512])
                Bsk, Bsv = Ra.b, Rb_.b
                rows = 128 if (prompt or t < NT - 1) else 64
                from_cache = (not prompt) and t < NT - 1
                if not from_cache:
                    if prompt:
                        S.dma("pool", o_nbk[t * 128:(t + 1) * 128, :], st_k, rbuf=Bsk, is_output=True)
                        S.dma("pool", o_nbv[t * 128:(t + 1) * 128, :], st_v, rbuf=Bsv, is_output=True)
                    else:
                        S.dma("pool", s_nbk[:, :], st_k[:64, :], rbuf=Bsk, is_output=True)
                        S.dma("pool", s_nbv[:, :], st_v[:64, :], rbuf=Bsv, is_output=True)
                S.op("act", lambda e: e.activation(out=kbf[:rows, :], in_=st_k[:rows, :], func=AF.Copy), reads=[Bsk], writes=[Rbf.b])
                to_T(kbf, Rbf.b, rows, 4, kbT, RkbT.b, t * 128, 4, eng="act")
                S.op("dve", lambda e: e.tensor_copy(out=vb[:rows, t, :], in_=st_v[:rows, :]), reads=[Bsv], writes=[Rvb.b])

            for step in range(NT + 2):
                if step < NT:
                    kfront(step)
                if step >= 2:
                    kback(step - 2)
            for r_ in list(Rst2.values()) + [Rjk]:
                AR.release(r_)
            AR.release(Rslab)

            Rqs = A(8 * 512 * 2, "QBslab"); qs = Rqs.view(BF16, [8, 512])
            S.dma("pool", qs, w_in_v[:, :, C_QB:C_QB + 512], wbuf=Rqs.b)
            nq = 256 if prompt else 64
            HG = 512 // nq
            RqbT = [A(8 * 256 * 2, f"qbT{i}") for i in range(2)]
            for r_ in RqbT:
                S.op("pool", lambda e, r_=r_: e.memset(r_.view(BF16, [r_.size // 2]), 0.0), writes=[r_.b])
            Re = [A(512 * 4, f"e{i}") for i in range(2)]
            Rsp = [A(512 * 2, f"sp{i}") for i in range(2)]
            RPt = [A(512 * 2, f"PtB{i}") for i in range(2)]
            RSl = [A(512 * 2, f"Sl{i}") for i in range(2)]
            st_q = Rst["c"].view(F32, [512]); Bsq = Rst["c"].b
            qbf = Rbf2.view(BF16, [512])
            nblk = NOWN // 2 if prompt else 1
            gcount = [0]

            def qside(j):
                Rq = RqbT[j % 2]; qbT = Rq.view(BF16, [4, 2, 256])
                for w in range(2 if prompt else 1):
                    m = 2 * j + w
                    rows = 128 if prompt else 64
                    Rx = Rxs[m % 2]; xs = Rx.view(F32, [1024])
                    src = x_own[m * 128:(m + 1) * 128, :] if prompt else x_s[:, :]
                    S.dma("sp", xs[:rows, :], src, wbuf=Rx.b)
                    RH = RhT[m % 2]; hT = RH.view(BF16, [8, 128])
                    norm_tile(xs[:rows, :], Rx.b, rows, A1v, B1v, jmod, hT, RH, 4, 5)
                    proj(hT, RH, rows, qs, Rqs.b, 0, 512, 1)
                    S.op("act", lambda e: e.activation(out=st_q[:rows, :], in_=bankf(1)[:rows, :], func=AF.Copy), reads=[Bbank[1]], writes=[Bsq])
                    S.op("pool", lambda e: e.tensor_scalar(out=qbf[:rows, :], in0=st_q[:rows, :], scalar1=0.125, scalar2=None, op0=ALU.mult), reads=[Bsq], writes=[Rbf2.b])
                    to_T(qbf, Rbf2.b, rows, 4, qbT, Rq.b, w * 128, 1, eng="dve", zpad=True)

            def attn(j):
                Rq = RqbT[j % 2]; qbT = Rq.view(BF16, [4, 2, 256])
                NKB = 4 * (j + 1) if prompt else NT
                ND = 4 if prompt else 1
                tok0 = j * 256 if prompt else NOWN * 128
                n = HG * nq
                for g in range(8 // HG):
                    gi = gcount[0]
                    gcount[0] += 1
                    bo = 6 + (gi % 2)
                    RS = RSl[gi % 2]; Sl = RS.view(BF16, [512])
                    S.op("pool", lambda e: e.memset(Sl[:, 0:n], 0.0), writes=[RS.b])
                    order = list(range(NKB - 1, -1, -1))

                    def zmm(bk_, kb, diag, b):
                        for hs in range(HG):
                            h = g * HG + hs
                            pr, hh = h // 2, h % 2
                            S.op("pe", lambda e, hs=hs, pr=pr, hh=hh: e.matmul(bankf(bk_)[:, hs * nq:(hs + 1) * nq], kbT[:, pr, kb * 128:(kb + 1) * 128], qbT[:, pr, hh, 0:nq], start=(hs == 0), stop=False, skip_group_check=True),
                                 reads=[RkbT.b, Rq.b], writes=[Bbank[bk_]])
                        if diag:
                            for hs in range(HG):
                                if prompt:
                                    for w in range(2):
                                        S.op("pe", lambda e, hs=hs, w=w: e.matmul(bankf(bk_)[:, hs * nq + w * 128:hs * nq + (w + 1) * 128], sbm[:, b, w, :], identb[:, :],
                                                                                start=False, stop=False, skip_group_check=True),
                                             reads=[Bc], writes=[Bbank[bk_]])
                                else:
                                    S.op("pe", lambda e, hs=hs: e.matmul(bankf(bk_)[:, hs * nq:(hs + 1) * nq], sbms[0:64, :], identb[0:64, 0:64],
                                                                       start=False, stop=False, skip_group_check=True),
                                         reads=[Bc], writes=[Bbank[bk_]])

                    def st0(kb, u):
                        zmm(0 + (u % 2), kb, kb >= NKB - ND, kb - (NKB - ND))

                    def st1(kb, u):
                        ba = 0 + (u % 2); bb = 2 + (u % 2)
                        RE = Re[u % 2]; ev = RE.view(F32, [512])
                        RSP = Rsp[u % 2]; spv = RSP.view(BF16, [512])
                        S.op("act", lambda e: e.activation(out=ev[:, 0:n], in_=bankf(ba)[:, 0:n], func=AF.Exp), reads=[Bbank[ba]], writes=[RE.b])
                        S.op("act", lambda e: e.activation(out=spv[:, 0:n], in_=ev[:, 0:n], func=AF.Ln, bias=onef[:, 0:1], scale=1.0), reads=[RE.b, Bonef], writes=[RSP.b])
                        zmm(bb, kb, kb >= NKB - ND, kb - (NKB - ND))
                        last = (u == 0)
                        S.op("pe", lambda e: e.matmul(bankf(bb)[:, 0:n], lneg[:, :], spv[:, 0:n], start=False, stop=last, skip_group_check=True), reads=[Bc, RSP.b], writes=[Bbank[bb]])
                        if u > 0:
                            S.op("pe", lambda e: e.matmul(bankf(bb)[:, 0:n], oneg[:, :], Sl[:, 0:n], start=False, stop=True, skip_group_check=True), reads=[Bc, RS.b], writes=[Bbank[bb]])
                        if u < NKB - 1:
                            S.op("pool", lambda e: e.tensor_tensor(out=Sl[:, 0:n], in0=Sl[:, 0:n], in1=spv[:, 0:n], op=ALU.add), reads=[RS.b, RSP.b], writes=[RS.b])

                    def st2(kb, u):
                        bb = 2 + (u % 2)
                        RP = RPt[u % 2]; Pt = RP.view(BF16, [512])
                        RSP = Rsp[u % 2]; spv = RSP.view(BF16, [512])
                        S.op("act", lambda e: e.activation(out=Pt[:, 0:n], in_=bankf(bb)[:, 0:n], func=AF.Exp), reads=[Bbank[bb]], writes=[RP.b])
                        for hs in range(HG):
                            h = g * HG + hs
                            S.op("pe", lambda e, hs=hs, h=h: e.matmul(bankf(bo)[64:128, hs * nq:(hs + 1) * nq], vb[:, kb, h * 64:(h + 1) * 64], Pt[:, hs * nq:(hs + 1) * nq],
                                                                    start=(u == 0 and hs == 0), stop=(u == NKB - 1), skip_group_check=True),
                                 reads=[Rvb.b, RP.b], writes=[Bbank[bo]])

                    for u in range(NKB + 2):
                        if u < NKB:
                            st0(order[u], u)
                        if 1 <= u <= NKB:
                            st1(order[u - 1], u - 1)
                        if u >= 2:
                            st2(order[u - 2], u - 2)
                    S.op("dve", lambda e, g=g: e.tensor_copy(out=oT[64:128, g * HG:(g + 1) * HG, tok0:tok0 + nq],
                                                            in_=bankf(bo)[64:128, 0:n].rearrange("p (a b) -> p a b", a=HG)),
                         reads=[Bbank[bo]], writes=[RoT.buf("b")])

            qside(0)
            for j in range(nblk):
                if j + 1 < nblk:
                    qside(j + 1)
                attn(j)
            for r in [Rqs] + RqbT + Re + Rsp + RPt + RSl + [RkbT, Rvb]:
                AR.release(r)

        def early():
            S.finish()
            build.info = dict(peak=AR.peak, nsem=S.nsem, cnt=dict(S.cnt))
            return nc, es
        if upto <= 0:
            return early()
        mixer_A("p")
        if upto <= 1:
            return early()
        mixer_A("s")
        if upto <= 2:
            return early()
        mixer_B("p")
        if upto <= 3:
            return early()
        mixer_B("s")
        if upto <= 4:
            return early()

        phase0(True)
        RG1 = GR["RG1"]; RG2 = GR["RG2"]
        G1 = RG1.view(F32, [2, 1024]); G2 = RG2.view(F32, [2, 1024])
        NTOK = TOWN
        tiles = [(m, 128, m * 128, 0) for m in range(NOWN)] + [(NOWN, 64, NOWN * 128, 1)]
        Rh1 = A(8 * NTOK * 2, "h1T"); h1T = Rh1.view(BF16, [8, NTOK])
        for (m, rows, tok0, j) in tiles:
            Rx = Rxs[m % 2]; xs = Rx.view(F32, [1024])
            src = x_own[m * 128:(m + 1) * 128, :] if j == 0 else x_s[:, :]
            S.dma("sp", xs[:rows, :], src, wbuf=Rx.b)
            norm_tile(xs[:rows, :], Rx.b, rows, A1v, B1v, j, h1T[:, :, tok0:tok0 + rows], Rh1, 0, 1)
        Rmg = A(8 * NTOK * 2, "mergedT"); mergedT = Rmg.view(BF16, [8, NTOK])
        Rgs = [A(8 * 256 * 2, f"gslab{i}") for i in range(2)]
        Rwu = [A(8 * 128 * 2, f"wu{i}") for i in range(2)]
        Rga = [A(512 * 4, f"ga{i}") for i in range(2)]
        Rgb = [A(512 * 4, f"gb{i}") for i in range(2)]
        Rm1 = [A(512 * 4, f"m1{i}") for i in range(2)]
        Rm2 = [A(512 * 4, f"m2{i}") for i in range(2)]
        chunks = [(c * 512, 512) for c in range(4)] + [(2048, 64)]

        def mgb(t0, n):
            return [Rmg.buf(m) for m in range(t0 // 128, (t0 + n + 127) // 128)]
        ci = 0
        for i in range(8):
            Rg = Rgs[i % 2]; gsl = Rg.view(BF16, [8, 256])
            S.dma("pool", gsl[:, :, 0:128], w_in_v[:, :, C_G + i * 128:C_G + (i + 1) * 128], wbuf=Rg.b)
            S.dma("pool", gsl[:, :, 128:256], w_in_v[:, :, C_G + 1024 + i * 128:C_G + 1024 + (i + 1) * 128], wbuf=Rg.b, more=True)
            Rw = Rwu[i % 2]; wu = Rw.view(BF16, [8, 128])
            S.dma("pool", wu, w_up[:, :, i * 128:(i + 1) * 128], wbuf=Rw.b)
            for (t0, n) in chunks:
                pb = (ci % 2) * 4
                ga = Rga[ci % 2].view(F32, [512]); gb = Rgb[ci % 2].view(F32, [512])
                m1 = Rm1[ci % 2].view(F32, [512]); m2 = Rm2[ci % 2].view(F32, [512])
                for k in range(8):
                    S.op("pe", lambda e, k=k: e.matmul(bankf(pb)[:, 0:n], gsl[:, k, 0:128], h1T[:, k, t0:t0 + n], start=(k == 0), stop=(k == 7)), reads=[Rg.b, Rh1.buf(k)], writes=[Bbank[pb]])
                for k in range(8):
                    S.op("pe", lambda e, k=k: e.matmul(bankf(pb + 1)[:, 0:n], gsl[:, k, 128:256], h1T[:, k, t0:t0 + n], start=(k == 0), stop=(k == 7)), reads=[Rg.b, Rh1.buf(k)], writes=[Bbank[pb + 1]])
                for h in range(8):
                    S.op("pe", lambda e, h=h: e.matmul(bankf(pb + 2)[:, 0:n], wu[0:64, h, :], oT[0:64, h, t0:t0 + n], start=(h == 0), stop=(h == 7)), reads=[Rw.b, RoT.buf("a")], writes=[Bbank[pb + 2]])
                for h in range(8):
                    S.op("pe", lambda e, h=h: e.matmul(bankf(pb + 3)[:, 0:n], wu[64:128, h, :], oT[64:128, h, t0:t0 + n], start=(h == 0), stop=(h == 7)), reads=[Rw.b, RoT.buf("b")], writes=[Bbank[pb + 3]])
                S.op("act", lambda e: e.activation(out=ga[:, 0:n], in_=bankf(pb)[:, 0:n], func=AF.Sigmoid), reads=[Bbank[pb]], writes=[Rga[ci % 2].b])
                S.op("act", lambda e: e.activation(out=gb[:, 0:n], in_=bankf(pb + 1)[:, 0:n], func=AF.Sigmoid), reads=[Bbank[pb + 1]], writes=[Rgb[ci % 2].b])
                S.op("dve", lambda e: e.tensor_tensor(out=m1[:, 0:n], in0=bankf(pb + 2)[:, 0:n], in1=ga[:, 0:n], op=ALU.mult), reads=[Bbank[pb + 2], Rga[ci % 2].b], writes=[Rm1[ci % 2].b])
                S.op("dve", lambda e: e.tensor_tensor(out=m2[:, 0:n], in0=bankf(pb + 3)[:, 0:n], in1=gb[:, 0:n], op=ALU.mult), reads=[Bbank[pb + 3], Rgb[ci % 2].b], writes=[Rm2[ci % 2].b])
                S.op("pool", lambda e, i=i: e.tensor_tensor(out=mergedT[:, i, t0:t0 + n], in0=m1[:, 0:n], in1=m2[:, 0:n], op=ALU.add), reads=[Rm1[ci % 2].b, Rm2[ci % 2].b], writes=mgb(t0, n))
                ci += 1
        for r in Rgs + Rwu + Rga + Rgb + Rm1 + Rm2 + [Rh1, RoT]:
            AR.release(r)

        if upto <= 5:
            return early()
        for r in [Rxn] + RhT + list(Rst.values()) + [Rbf, Rbf2, Rt1]:
            AR.release(r)
        Rwo = A(8 * 1024 * 2, "wout"); wo = Rwo.view(BF16, [8, 1024])
        S.dma("pool", wo, w_out_v, wbuf=Rwo.b)
        Ry = [A(1024 * 4, f"y{m}") for m in range(17)]
        yv = [r.view(F32, [1024]) for r in Ry]
        h2T = mergedT
        Rh2fs = [A(8 * 128 * 4, f"h2Tf{i}") for i in range(2)]
        Rcomb = A(17 * 32 * 4, "comb"); comb = Rcomb.view(F32, [17, 32])
        Rtys = [A(1024 * 4, f"ty{i}") for i in range(2)]
        Rxnfs = [A(1024 * 4, f"xnf{i}") for i in range(2)]
        NTL = 17
        Rrs = A(NTL * 160 * 4, "route")
        _ro = [0]

        def rsl(n):
            v = Rrs.view(F32, [NTL, n], _ro[0]) if n > 1 else Rrs.view(F32, [NTL], _ro[0])
            _ro[0] += NTL * n * 4
            return v
        lgall = rsl(36); gmxa = rsl(1); goha = rsl(4); dga = rsl(4); gexa = rsl(4); gsuma = rsl(1); pga = rsl(1)
        eta = rsl(32); esela = rsl(8); m1a = rsl(1); oh1a = rsl(8); e2a = rsl(8); m2a = rsl(1); oh2a = rsl(8)
        dda = rsl(1); ex2a = rsl(1); w1a = rsl(1); w2a = rsl(1); c8a = rsl(8)
        Brt = Rrs.b
        S.op("pool", lambda e: e.memset(lgall, 0.0), writes=[Brt])
        for (m, rows, tok0, j) in tiles:
            yb = 0 if m % 2 == 0 else 2
            Rh2f = Rh2fs[m % 2]; h2f = Rh2f.view(F32, [8, 128])
            Rty = Rtys[m % 2]; ty = Rty.view(F32, [1024])
            XF["v"] = Rxnfs[m % 2].view(F32, [1024]); XF["b"] = Rxnfs[m % 2].b
            for half in range(2):
                for k in range(8):
                    S.op("pe", lambda e, k=k, half=half: e.matmul(bankf(yb + half)[:rows, :], mergedT[:, k, tok0:tok0 + rows], wo[:, k, half * 512:(half + 1) * 512], start=(k == 0), stop=(k == 7)),
                         reads=[Rmg.buf(m), Rwo.b], writes=[Bbank[yb + half]])
            Rx = Rxs[m % 2]; xs = Rx.view(F32, [1024])
            src = x_own[m * 128:(m + 1) * 128, :] if j == 0 else x_s[:, :]
            S.dma("sp", xs[:rows, :], src, wbuf=Rx.b)
            Bym = Ry[m].b
            for half in range(2):
                S.op("dve", lambda e, half=half: e.tensor_tensor(out=ty[:rows, half * 512:(half + 1) * 512], in0=bankf(yb + half)[:rows, :], in1=G1[:rows, j, half * 512:(half + 1) * 512], op=ALU.mult),
                     reads=[Bbank[yb + half], RG1.b], writes=[Rty.b])
            S.op("pool", lambda e, m=m: e.tensor_tensor(out=yv[m][:rows, :], in0=ty[:rows, :], in1=xs[:rows, :], op=ALU.add), reads=[Rty.b, Rx.b], writes=[Bym])
            tb1 = 4 if m % 2 == 0 else 6
            norm_tile(yv[m][:rows, :], Bym, rows, A2v, B2v, j, h2f, Rh2f, tb1, tb1 + 1, fp32=True)
            S.op("pool", lambda e: e.tensor_copy(out=h2T[:, :, tok0:tok0 + rows], in_=h2f[:, :, :rows]), reads=[Rh2f.buf(k_) for k_ in range(8)], writes=[Rmg.buf(m)])
            for k in range(8):
                S.op("pe", lambda e, k=k: e.matmul(bankf(tb1)[:rows, 0:36], h2f[:, k, :rows], wr_sb[:, k, :], start=(k == 0), stop=(k == 7)), reads=[Rh2f.buf(k), Bc2], writes=[Bbank[tb1]])
            S.op("dve", lambda e, m=m: e.tensor_tensor(out=lgall[:rows, m, :], in0=bankf(tb1)[:rows, 0:36], in1=brr[:rows, :], op=ALU.add), reads=[Bbank[tb1], Bc2], writes=[Brt])
        P_ = 128

        def dv(fn):
            S.op("dve", fn, reads=[Brt], writes=[Brt])
        lgg = lgall[:, :, 0:4]
        lge = lgall[:, :, 4:36].rearrange("p t (g x) -> p t g x", g=4)
        dv(lambda e: e.reduce_max(out=gmxa, in_=lgg, axis=AX.X))
        dv(lambda e: e.tensor_tensor(out=goha, in0=lgg, in1=gmxa.unsqueeze(2).to_broadcast([P_, NTL, 4]), op=ALU.is_ge))
        dv(lambda e: e.tensor_tensor(out=dga, in0=lgg, in1=gmxa.unsqueeze(2).to_broadcast([P_, NTL, 4]), op=ALU.subtract))
        S.op("act", lambda e: e.activation(out=gexa, in_=dga, func=AF.Exp), reads=[Brt], writes=[Brt])
        dv(lambda e: e.reduce_sum(out=gsuma, in_=gexa, axis=AX.X))
        dv(lambda e: e.reciprocal(out=pga, in_=gsuma))
        etv = eta.rearrange("p t (g x) -> p t g x", g=4)
        dv(lambda e: e.tensor_tensor(out=etv, in0=lge, in1=goha.unsqueeze(3).to_broadcast([P_, NTL, 4, 8]), op=ALU.mult))
        dv(lambda e: e.reduce_sum(out=esela, in_=eta.rearrange("p t (g x) -> p t x g", g=4), axis=AX.X))
        dv(lambda e: e.reduce_max(out=m1a, in_=esela, axis=AX.X))
        dv(lambda e: e.tensor_tensor(out=oh1a, in0=esela, in1=m1a.unsqueeze(2).to_broadcast([P_, NTL, 8]), op=ALU.is_ge))
        dv(lambda e: e.scalar_tensor_tensor(out=e2a, in0=oh1a, scalar=-1e30, in1=esela, op0=ALU.mult, op1=ALU.add))
        dv(lambda e: e.reduce_max(out=m2a, in_=e2a, axis=AX.X))
        dv(lambda e: e.tensor_tensor(out=oh2a, in0=e2a, in1=m2a.unsqueeze(2).to_broadcast([P_, NTL, 8]), op=ALU.is_ge))
        dv(lambda e: e.tensor_tensor(out=dda, in0=m2a, in1=m1a, op=ALU.subtract))
        S.op("act", lambda e: e.activation(out=ex2a, in_=dda, func=AF.Exp), reads=[Brt], writes=[Brt])
        dv(lambda e: e.tensor_scalar(out=w1a, in0=ex2a, scalar1=1.0, scalar2=None, op0=ALU.add))
        dv(lambda e: e.reciprocal(out=w1a, in_=w1a))
        dv(lambda e: e.tensor_tensor(out=w1a, in0=w1a, in1=pga, op=ALU.mult))
        dv(lambda e: e.tensor_tensor(out=w2a, in0=w1a, in1=ex2a, op=ALU.mult))
        dv(lambda e: e.tensor_tensor(out=oh1a, in0=oh1a, in1=w1a.unsqueeze(2).to_broadcast([P_, NTL, 8]), op=ALU.mult))
        dv(lambda e: e.tensor_tensor(out=oh2a, in0=oh2a, in1=w2a.unsqueeze(2).to_broadcast([P_, NTL, 8]), op=ALU.mult))
        dv(lambda e: e.tensor_tensor(out=c8a, in0=oh1a, in1=oh2a, op=ALU.add))
        S.op("dve", lambda e: e.tensor_tensor(out=comb.rearrange("p t (g x) -> p t g x", g=4), in0=goha.unsqueeze(3).to_broadcast([P_, NTL, 4, 8]),
                                             in1=c8a.unsqueeze(2).to_broadcast([P_, NTL, 4, 8]), op=ALU.mult), reads=[Brt], writes=[Rcomb.b])
        for r in [Rwo, Rrs] + Rh2fs + Rtys + Rxnfs + Rxs + [Rtmp, RG1]:
            AR.release(r)

        if upto <= 6:
            return early()
        Rgu = [A(8 * 512 * 2, f"gu{i}") for i in range(4)]
        Rdn = [A(2 * 1024 * 2, f"dn{i}") for i in range(4)]
        Rdp = [A(2 * 1024 * 2, f"dnp{i}") for i in range(4)]
        Rsg = [A(256 * 4, f"sg{i}") for i in range(2)]
        Rac = [A(256 * 2, f"ac{i}") for i in range(2)]
        RaT = [A(256 * 2, f"aT{i}") for i in range(2)]
        Rys = A(1024 * 4, "ys"); ysv = Rys.view(F32, [1024])
        G2b = G2[:, 0, :]
        NGRP = NEXP // 2
        loaded = set()

        def load_group(grp):
            if grp in loaded or grp >= NGRP:
                return
            loaded.add(grp)
            for ei in range(2):
                ex = grp * 2 + ei
                sl = (grp % 2) * 2 + ei
                gu = Rgu[sl].view(BF16, [8, 512])
                S.dma("pool", gu[:, :, 0:256], w_eg[ex].rearrange("(kc p) n -> p kc n", p=128), wbuf=Rgu[sl].b)
                S.dma("pool", gu[:, :, 256:512], w_eu[ex].rearrange("(kc p) n -> p kc n", p=128), wbuf=Rgu[sl].b, more=True)
                dn = Rdn[sl].view(BF16, [2, 1024])
                S.dma("pool", dn, w_ed[ex].rearrange("(fc p) n -> p fc n", p=128), wbuf=Rdn[sl].b)

        def scale_group(grp):
            for ei in range(2):
                sl = (grp % 2) * 2 + ei
                dn = Rdn[sl].view(BF16, [2, 1024]); dp = Rdp[sl].view(BF16, [2, 1024])
                S.op("dve", lambda e: e.tensor_tensor(out=dp, in0=dn, in1=G2b.unsqueeze(1).to_broadcast([128, 2, 1024]), op=ALU.mult), reads=[Rdn[sl].b, RG2.b], writes=[Rdp[sl].b])

        units = [(grp, m, rows, tok0, j, ei) for grp in range(NGRP) for (m, rows, tok0, j) in tiles for ei in range(2)]

        def stA(u):
            grp, m, rows, tok0, j, ei = units[u]
            sl = (grp % 2) * 2 + ei
            if m == 0 and ei == 0:
                load_group(grp)
                scale_group(grp)
            if m == 8 and ei == 0:
                load_group(grp + 1)
            gu = Rgu[sl].view(BF16, [8, 512])
            gb_ = u % 2
            for k in range(8):
                S.op("pe", lambda e, k=k: e.matmul(bankf(gb_)[:rows, :], h2T[:, k, tok0:tok0 + rows], gu[:, k, :], start=(k == 0), stop=(k == 7)), reads=[Rmg.buf(m), Rgu[sl].b], writes=[Bbank[gb_]])
            sg = Rsg[u % 2].view(F32, [256]); ac = Rac[u % 2].view(BF16, [256])
            ex = grp * 2 + ei
            S.op("act", lambda e: e.activation(out=sg[:rows, :], in_=bankf(gb_)[:rows, 0:256], func=AF.Silu), reads=[Bbank[gb_]], writes=[Rsg[u % 2].b])
            S.op("dve", lambda e: e.scalar_tensor_tensor(out=ac[:rows, :], in0=sg[:rows, :], scalar=comb[:rows, m, ex:ex + 1], in1=bankf(gb_)[:rows, 256:512], op0=ALU.mult, op1=ALU.mult),
                 reads=[Rsg[u % 2].b, Rcomb.b, Bbank[gb_]], writes=[Rac[u % 2].b])

        def stB(u):
            grp, m, rows, tok0, j, ei = units[u]
            ac = Rac[u % 2].view(BF16, [256]); aT = RaT[u % 2].view(BF16, [2, 128])
            tb = 2 + (u % 2)
            tpv = bankb(tb)
            for f in range(2):
                S.op("pe", lambda e, f=f: e.transpose(tpv[:, f * 128:f * 128 + rows], ac[:rows, f * 128:(f + 1) * 128], identb[:rows, :rows]), reads=[Rac[u % 2].b, Bc], writes=[Bbank[tb]])
            S.op("act", lambda e: e.activation(out=aT[:, :, :rows], in_=tpv[:, 0:256].rearrange("p (a b) -> p a b", a=2)[:, :, :rows], func=AF.Copy), reads=[Bbank[tb]], writes=[RaT[u % 2].b])

        def stC(u):
            grp, m, rows, tok0, j, ei = units[u]
            sl = (grp % 2) * 2 + ei
            aT = RaT[u % 2].view(BF16, [2, 128])
            yb = 4 if m % 2 == 0 else 6
            dnx = Rdp[sl].view(BF16, [2, 1024]) if j == 0 else Rdn[sl].view(BF16, [2, 1024])
            Bdnx = Rdp[sl].b if j == 0 else Rdn[sl].b
            for half in range(2):
                for f in range(2):
                    S.op("pe", lambda e, f=f, half=half: e.matmul(bankf(yb + half)[:rows, :], aT[:, f, :rows], dnx[:, f, half * 512:(half + 1) * 512],
                                                                start=(ei == 0 and f == 0), stop=(ei == 1 and f == 1), skip_group_check=True),
                         reads=[RaT[u % 2].b, Bdnx], writes=[Bbank[yb + half]])
            if ei == 1:
                Bym = Ry[m].b
                for half in range(2):
                    ysl = yv[m][:rows, half * 512:(half + 1) * 512]
                    if j == 0:
                        S.op("dve", lambda e, half=half, ysl=ysl: e.tensor_tensor(out=ysl, in0=bankf(yb + half)[:rows, :], in1=ysl, op=ALU.add), reads=[Bbank[yb + half], Bym], writes=[Bym])
                    else:
                        S.op("dve", lambda e, half=half: e.tensor_tensor(out=ysv[:rows, half * 512:(half + 1) * 512], in0=bankf(yb + half)[:rows, :], in1=G2[:rows, 1, half * 512:(half + 1) * 512], op=ALU.mult),
                             reads=[Bbank[yb + half], RG2.b], writes=[Rys.b])
                        S.op("pool", lambda e, half=half, ysl=ysl: e.tensor_tensor(out=ysl, in0=ysl, in1=ysv[:rows, half * 512:(half + 1) * 512], op=ALU.add), reads=[Rys.b, Bym], writes=[Bym])

        NU = len(units)
        load_group(0)
        for step in range(NU + 2):
            if step < NU:
                stA(step)
            if 1 <= step <= NU:
                stB(step - 1)
            if step >= 2:
                stC(step - 2)
        for (m, rows, tok0, j) in tiles:
            dst = y_own[m * 128:(m + 1) * 128, :] if j == 0 else y_s[:, :]
            S.dma("sp", dst, yv[m][:rows, :], rbuf=Ry[m].b, is_output=True)

    except Stop:
        pass
    S.finish()
    build.info = dict(peak=AR.peak, nsem=S.nsem, cnt=dict(S.cnt))
    return nc, es


def _consts(par):
    bf = ml_dtypes.bfloat16
    c = {}
    c["c_identb"] = np.eye(128, dtype=np.float32).astype(bf)
    c["c_identf"] = np.eye(128, dtype=np.float32)
    jj, ss = np.meshgrid(np.arange(128), np.arange(128), indexing="ij")
    c["c_lneg"] = np.where(jj >= ss, -1.0, 0.0).astype(np.float32).astype(bf)
    c["c_oneg"] = np.full((128, 128), -1.0, np.float32).astype(bf)
    T = TPAR[par]
    sbm = np.zeros((128, 4, 2, 128), np.float32)
    tq, sk = np.meshgrid(np.arange(128), np.arange(128), indexing="ij")
    for b in range(4):
        for w in range(2):
            r = T[w]
            if b < r:
                vis = np.ones((128, 128), bool)
            elif b == r:
                vis = sk < tq
            else:
                vis = np.zeros((128, 128), bool)
            sbm[:, b, w, :] = np.where(vis, 0.0, NEG)
    c["c_sbm"] = sbm.reshape(128, -1).astype(bf)
    tq, sk = np.meshgrid(np.arange(64), np.arange(128), indexing="ij")
    c["c_sbms"] = np.where(sk < tq, 0.0, NEG).astype(np.float32).astype(bf)
    adm = np.zeros((128, 2, 512), np.float32)
    t = np.arange(128)[:, None]
    s = np.arange(512)[None, :]
    for w in range(2):
        r = T[w]
        ok = (s // 64) <= (2 * r + t // 64)
        adm[:, w, :] = np.where(ok, 0.0, -1e30)
    c["c_adm"] = adm.reshape(128, -1)
    freqs = (np.float32(500000.0) ** (-np.arange(0, 16, 2, dtype=np.float32) / np.float32(16))).astype(np.float32)

    def rope_tab(pos):
        ang = pos.astype(np.float32)[..., None] * freqs[None, None, :]
        cs = np.cos(ang).astype(np.float32)
        sn = np.sin(ang).astype(np.float32)
        return np.concatenate([cs, cs, sn], axis=-1).astype(np.float32)
    p = np.arange(128)[:, None]
    c["c_ropep"] = rope_tab(np.arange(NTP)[None, :] * 128 + p).reshape(128, -1)
    own_g = np.array([4 * (m // 2) + T[m % 2] for m in range(NOWN)])
    c["c_ropeo"] = rope_tab(own_g[None, :] * 128 + p).reshape(128, -1)
    c["c_ropes"] = rope_tab(4096 + p).reshape(128, -1)
    c["c_pow2"] = np.tile((2.0 ** -np.arange(24, dtype=np.float64)).astype(np.float32)[None, :], (128, 1))
    return c, own_g


_CACHE = {}


def kernel(x_prompt, x_sample, cache_a_k, cache_a_v, cache_a_kidx, cache_b_k, cache_b_v, c_prompt, c_sample,
           w_ada, b_ada, norm1_g, w_in, qnorm_g, knorm_g, w_up_a, w_up_b, w_out, norm2_g,
           w_rg, b_rg, w_re, b_re, w_e_gate, w_e_up, w_e_down):
    f = lambda a: np.ascontiguousarray(np.asarray(a, dtype=np.float32))
    x_prompt = f(x_prompt); x_sample = f(x_sample)
    if "nc" not in _CACHE:
        import os
        _CACHE["nc"] = build(float(os.environ.get("K_UPTO", "9")))
    nc, _es = _CACHE["nc"]
    shared = {
        "w_ada": f(w_ada[0]), "b_adaT": f(np.asarray(b_ada[0]).reshape(48, 128).T), "b_ada": f(np.asarray(b_ada[0]).reshape(1, -1)),
        "n1gT": f(np.asarray(norm1_g[0]).reshape(8, 128).T), "n2gT": f(np.asarray(norm2_g[0]).reshape(8, 128).T),
        "gq": f(np.asarray(qnorm_g[0]).reshape(1, 64)), "gk": f(np.asarray(knorm_g[0]).reshape(1, 64)),
        "w_in": f(w_in[0]),
        "w_up": f(np.concatenate([np.asarray(w_up_a[0]).reshape(8, 64, D).transpose(1, 0, 2), np.asarray(w_up_b[0]).reshape(8, 64, D).transpose(1, 0, 2)], axis=0)),
        "w_out": f(w_out[0]),
        "w_r": f(np.concatenate([np.asarray(w_rg[0]), np.asarray(w_re[0])], axis=1)),
        "b_r": f(np.concatenate([np.asarray(b_rg[0]), np.asarray(b_re[0])]).reshape(1, 36)),
        "w_eg": f(w_e_gate[0]), "w_eu": f(w_e_up[0]), "w_ed": f(w_e_down[0]),
    }
    in_maps = []
    owns = []
    for c in range(8):
        b, par = c // 2, c % 2
        cst, own_g = _consts(par)
        owns.append(own_g)
        rows = np.concatenate([np.arange(g * 128, (g + 1) * 128) for g in own_g])
        m = dict(shared)
        m.update(cst)
        m["x_p"] = x_prompt[b]
        m["x_own"] = np.ascontiguousarray(x_prompt[b][rows])
        m["x_s"] = x_sample[c]
        m["ca_k"] = f(np.asarray(cache_a_k[0, c]).reshape(4096, 512)); m["ca_v"] = f(np.asarray(cache_a_v[0, c]).reshape(4096, 512))
        m["ca_ki"] = f(cache_a_kidx[0, c])
        m["cb_k"] = f(np.asarray(cache_b_k[0, c]).reshape(4096, 512)); m["cb_v"] = f(np.asarray(cache_b_v[0, c]).reshape(4096, 512))
        cc_ = np.stack([np.asarray(c_prompt[b]), np.asarray(c_sample[c])], axis=1)
        m["cT"] = f(cc_.reshape(8, 128, 2).transpose(1, 0, 2).reshape(128, 16))
        in_maps.append(m)
    import os
    ncore = int(os.environ.get("K_CORES", "8"))
    res = run_bass_kernel_spmd(nc, in_maps[:ncore], core_ids=list(range(ncore)))
    R = list(res.results) + [res.results[0]] * (8 - ncore)
    y_prompt = np.zeros((4, 4096, D), np.float32)
    y_sample = np.zeros((8, 64, D), np.float32)
    outs_p = {k: np.zeros((1, 4) + s, np.float32) for k, s in
              (("o_nak", (4096, 8, 64)), ("o_nav", (4096, 8, 64)), ("o_naki", (4096, 64)), ("o_nbk", (4096, 8, 64)), ("o_nbv", (4096, 8, 64)))}
    outs_s = {k: np.zeros((1, 8) + s, np.float32) for k, s in
              (("s_nak", (64, 8, 64)), ("s_nav", (64, 8, 64)), ("s_naki", (64, 64)), ("s_nbk", (64, 8, 64)), ("s_nbv", (64, 8, 64)))}
    for c in range(8):
        b = c // 2
        yo = R[c]["y_own"]
        for mi, g in enumerate(owns[c]):
            y_prompt[b, g * 128:(g + 1) * 128] = yo[mi * 128:(mi + 1) * 128]
        y_sample[c] = R[c]["y_s"]
        if c % 2 == 0:
            for k in outs_p:
                outs_p[k][0, b] = R[c][k].reshape(outs_p[k].shape[2:])
        for k in outs_s:
            outs_s[k][0, c] = R[c][k].reshape(outs_s[k].shape[2:])
    return (y_prompt, y_sample, outs_p["o_nak"], outs_p["o_nav"], outs_p["o_naki"], outs_p["o_nbk"], outs_p["o_nbv"],
            outs_s["s_nak"], outs_s["s_nav"], outs_s["s_naki"], outs_s["s_nbk"], outs_s["s_nbv"])
```

```python
import numpy as np
import ml_dtypes
import contextlib
import concourse.bass as bass
import concourse.mybir as mybir
from concourse.bass_utils import run_bass_kernel_spmd

F32 = mybir.dt.float32
BF16 = mybir.dt.bfloat16
AF = mybir.ActivationFunctionType
ALU = mybir.AluOpType
AX = mybir.AxisListType

D = 1024
KC = 8
NTP = 32
NOWN = 16
NTS = 33
NKS = NTS * 128
TOWN = NOWN * 128 + 64
NIT = 14
NEG = -30000.0
TPAR = ((0, 3), (1, 2))
C_QA, C_KA, C_VA, C_QI, C_KI, C_WI, C_QB, C_KB, C_VB, C_G = 0, 512, 1024, 1536, 1792, 1856, 1860, 2372, 2884, 3396
NEXP = 32


class Buf:
    __slots__ = ("name", "wdeps", "rdeps", "dsem", "dcnt", "rsem", "rcnt", "excl")

    def __init__(self, name, inherit=(), excl=False):
        self.name = name
        self.excl = excl
        self.wdeps = set()
        self.rdeps = set(inherit)
        self.dsem = None
        self.dcnt = 0
        self.rsem = None
        self.rcnt = 0


class Sched:
    ENGS = ("pe", "act", "dve", "pool", "sp")

    def __init__(self, nc):
        self.nc = nc
        self.eng = {"pe": nc.tensor, "act": nc.scalar, "dve": nc.vector, "pool": nc.gpsimd, "sp": nc.sync}
        self.sem = {e: nc.alloc_semaphore(f"q_{e}") for e in self.ENGS}
        self.cnt = {e: 0 for e in self.ENGS}
        self.seen = {e: {} for e in self.ENGS}
        self.semobj = {}
        self.out_deps = set()
        self.free_dsems = []
        self.nsem = 0

    def _wait(self, e, deps):
        eng = self.eng[e]
        seen = self.seen[e]
        best = {}
        for (sem, val) in deps:
            k = id(sem)
            self.semobj[k] = sem
            if seen.get(k, 0) >= val:
                continue
            if best.get(k, 0) < val:
                best[k] = val
        for k, val in best.items():
            eng.wait_ge(self.semobj[k], val)
            seen[k] = val

    def op(self, e, fn, reads=(), writes=()):
        own = id(self.sem[e])
        deps = set()
        for b in reads:
            for d in b.wdeps:
                if id(d[0]) == own and e == "pe":
                    continue
                deps.add(d)
            if b.excl:
                for d in b.rdeps:
                    if id(d[0]) != own:
                        deps.add(d)
        for b in writes:
            for d in b.wdeps:
                if id(d[0]) != own:
                    deps.add(d)
            for d in b.rdeps:
                if id(d[0]) != own:
                    deps.add(d)
        self._wait(e, deps)
        ins = fn(self.eng[e])
        self.cnt[e] += 1
        ins.then_inc(self.sem[e], 1)
        d = (self.sem[e], self.cnt[e])
        for b in reads:
            b.rdeps = {x for x in b.rdeps if id(x[0]) != own} | {d}
        for b in writes:
            b.wdeps = {d}
            b.rdeps = set()
        return ins

    def _newsem(self, name):
        self.nsem += 1
        return self.nc.alloc_semaphore(f"{name}_{self.nsem}")

    def dma(self, e, out_ap, in_ap, wbuf=None, rbuf=None, more=False, is_output=False):
        deps = set()
        if wbuf is not None:
            if more and wbuf.dsem is not None:
                deps |= {d for d in wbuf.wdeps if id(d[0]) != id(wbuf.dsem)}
            else:
                deps |= wbuf.wdeps
            deps |= wbuf.rdeps
        if rbuf is not None:
            deps |= rbuf.wdeps
        self._wait(e, deps)
        ins = self.eng[e].dma_start(out=out_ap, in_=in_ap)
        if wbuf is not None:
            if wbuf.dsem is None:
                wbuf.dsem = self._newsem("d_" + wbuf.name)
            wbuf.dcnt += 16
            ins.then_inc(wbuf.dsem, 16)
            keep = set()
            if more:
                keep = {d for d in wbuf.wdeps if id(d[0]) != id(wbuf.dsem)}
            wbuf.wdeps = keep | {(wbuf.dsem, wbuf.dcnt)}
            wbuf.rdeps = set()
        elif rbuf is not None:
            if rbuf.rsem is None:
                rbuf.rsem = self._newsem("r_" + rbuf.name)
            rbuf.rcnt += 16
            ins.then_inc(rbuf.rsem, 16)
            d = (rbuf.rsem, rbuf.rcnt)
            rbuf.rdeps = {x for x in rbuf.rdeps if id(x[0]) != id(rbuf.rsem)} | {d}
            if is_output:
                self.out_deps = {x for x in self.out_deps if id(x[0]) != id(rbuf.rsem)} | {d}
        return ins

    def finish(self):
        deps = set(self.out_deps)
        for e in self.ENGS:
            if self.cnt[e] > 0:
                deps.add((self.sem[e], self.cnt[e]))
        self._wait("sp", deps)


class Stop(Exception):
    pass


class Region:
    def __init__(self, arena, off, size, name, inherit):
        self.arena = arena
        self.off = off
        self.size = size
        self.name = name
        self.inherit = inherit
        self.bufs = {}
        self.b = self.buf(None)

    def buf(self, key):
        if key not in self.bufs:
            self.bufs[key] = Buf(f"{self.name}_{key}", inherit=self.inherit)
        return self.bufs[key]

    def view(self, dt, dims, byte_off=0):
        esz = 2 if dt == BF16 else 4
        n = 1
        for x in dims:
            n *= x
        o = (self.off + byte_off) // 2
        assert byte_off + n * esz <= self.size, (self.name, byte_off, n, esz, self.size)
        ap = self.arena.t[:, o:o + n * esz // 2]
        if dt != BF16:
            ap = ap.bitcast(dt)
        if len(dims) == 2:
            ap = ap.rearrange("p (a b) -> p a b", a=dims[0])
        elif len(dims) == 3:
            ap = ap.rearrange("p (a b c) -> p a b c", a=dims[0], b=dims[1])
        return ap


class Arena:
    def __init__(self, t, nbytes):
        self.t = t
        self.nbytes = nbytes
        self.free = [(0, nbytes)]
        self.retired = []
        self.peak = 0

    def alloc(self, nbytes, name):
        nbytes = (nbytes + 63) // 64 * 64
        for i, (o, s) in enumerate(self.free):
            if s >= nbytes:
                if s == nbytes:
                    self.free.pop(i)
                else:
                    self.free[i] = (o + nbytes, s - nbytes)
                inh = set()
                for (a, b, deps) in self.retired:
                    if a < o + nbytes and b > o:
                        inh |= deps
                self.peak = max(self.peak, o + nbytes)
                return Region(self, o, nbytes, name, inh)
        raise RuntimeError(f"arena full allocating {name} {nbytes}; free={self.free}")

    def release(self, r):
        deps = set()
        for b in r.bufs.values():
            deps |= b.wdeps | b.rdeps
        self.retired = [(a, b, d) for (a, b, d) in self.retired if not (a >= r.off and b <= r.off + r.size)]
        self.retired.append((r.off, r.off + r.size, deps))
        self.free.append((r.off, r.size))
        self.free.sort()
        m = []
        for o, s in self.free:
            if m and m[-1][0] + m[-1][1] == o:
                m[-1] = (m[-1][0], m[-1][1] + s)
            else:
                m.append((o, s))
        self.free = m


def build(upto=9):
    nc = bass.Bass("TRN2", target_bir_lowering=False)
    S = Sched(nc)

    def din(name, shape, dt=F32):
        return nc.dram_tensor(name, list(shape), dt, kind="ExternalInput").ap()

    def dout(name, shape, dt=F32):
        return nc.dram_tensor(name, list(shape), dt, kind="ExternalOutput").ap()

    x_p = din("x_p", [4096, D]); x_own = din("x_own", [2048, D]); x_s = din("x_s", [64, D])
    ca_k = din("ca_k", [4096, 512]); ca_v = din("ca_v", [4096, 512]); ca_ki = din("ca_ki", [4096, 64])
    cb_k = din("cb_k", [4096, 512]); cb_v = din("cb_v", [4096, 512])
    cT = din("cT", [128, 16])
    w_ada = din("w_ada", [D, 6 * D]); b_adaT = din("b_adaT", [128, 48]); b_ada = din("b_ada", [1, 6 * D])
    n1gT = din("n1gT", [128, 8]); n2gT = din("n2gT", [128, 8]); gq = din("gq", [1, 64]); gk = din("gk", [1, 64])
    w_in = din("w_in", [D, 5444]); w_up = din("w_up", [128, 8, D]); w_out = din("w_out", [D, D])
    w_r = din("w_r", [D, 36]); b_r = din("b_r", [1, 36])
    w_eg = din("w_eg", [NEXP, D, 256]); w_eu = din("w_eu", [NEXP, D, 256]); w_ed = din("w_ed", [NEXP, 256, D])
    c_identb = din("c_identb", [128, 128], BF16); c_identf = din("c_identf", [128, 128])
    c_lneg = din("c_lneg", [128, 128], BF16); c_oneg = din("c_oneg", [128, 128], BF16)
    c_sbm = din("c_sbm", [128, 8 * 128], BF16); c_sbms = din("c_sbms", [64, 128], BF16)
    c_adm = din("c_adm", [128, 2 * 512])
    c_ropep = din("c_ropep", [128, NTP * 24]); c_ropeo = din("c_ropeo", [128, NOWN * 24]); c_ropes = din("c_ropes", [128, 24])
    c_pow2 = din("c_pow2", [128, 24])

    y_own = dout("y_own", [2048, D]); y_s = dout("y_s", [64, D])
    o_nak = dout("o_nak", [4096, 512]); o_nav = dout("o_nav", [4096, 512]); o_naki = dout("o_naki", [4096, 64])
    o_nbk = dout("o_nbk", [4096, 512]); o_nbv = dout("o_nbv", [4096, 512])
    s_nak = dout("s_nak", [64, 512]); s_nav = dout("s_nav", [64, 512]); s_naki = dout("s_naki", [64, 64])
    s_nbk = dout("s_nbk", [64, 512]); s_nbv = dout("s_nbv", [64, 512])

    w_in_v = w_in.rearrange("(kc p) n -> p kc n", p=128)
    w_ada_v = w_ada.rearrange("(kc p) n -> p kc n", p=128)
    w_out_v = w_out.rearrange("(kc p) n -> p kc n", p=128)
    w_r_v = w_r.rearrange("(kc p) n -> p kc n", p=128)

    def chk(x):
        if upto <= x:
            raise Stop()

    es = contextlib.ExitStack()
    ARENA_BYTES = 207 * 1024
    arena_t = es.enter_context(nc.sbuf_tensor("arena", [128, ARENA_BYTES // 2], BF16))
    AR = Arena(arena_t, ARENA_BYTES)
    banks = [es.enter_context(nc.psum_tensor(f"bank{i}", [128, 512], F32)) for i in range(8)]
    Bbank = [Buf(f"bank{i}", excl=True) for i in range(8)]

    def bankf(i):
        return banks[i][:, :]

    def bankb(i):
        return banks[i][:, :].bitcast(BF16)

    def A(nbytes, name):
        return AR.alloc(nbytes, name)

    Rc = A(128 * 2 * 3 + 128 * 4 + 1024 * 2 + 128 * 2 + 2 * 512 * 4 + 24 * 4 + 64, "consts")
    o = 0
    identb = Rc.view(BF16, [128], o); o += 256
    lneg = Rc.view(BF16, [128], o); o += 256
    oneg = Rc.view(BF16, [128], o); o += 256
    identf = Rc.view(F32, [128], o); o += 512
    sbm = Rc.view(BF16, [4, 2, 128], o); o += 2048
    sbms = Rc.view(BF16, [128], o); o += 256
    adm = Rc.view(F32, [2, 512], o); o += 4096
    pow2 = Rc.view(F32, [24], o); o += 96
    Bc = Rc.b
    S.dma("sp", identb, c_identb[:, :], wbuf=Bc)
    S.dma("sp", lneg, c_lneg[:, :], wbuf=Bc, more=True)
    S.dma("sp", oneg, c_oneg[:, :], wbuf=Bc, more=True)
    S.dma("sp", identf, c_identf[:, :], wbuf=Bc, more=True)
    S.dma("sp", sbm, c_sbm.rearrange("p (a b c) -> p a b c", a=4, b=2), wbuf=Bc, more=True)
    S.dma("sp", sbms[0:64, :], c_sbms[:, :], wbuf=Bc, more=True)
    S.dma("sp", adm, c_adm.rearrange("p (a b) -> p a b", a=2), wbuf=Bc, more=True)
    S.dma("sp", pow2, c_pow2[:, :], wbuf=Bc, more=True)

    Rc2 = A(NTP * 24 * 4 + NOWN * 24 * 4 + 24 * 4 + 64 * 4 * 2 + 36 * 4 + 8 * 36 * 4 + 16 * 4 * 3 + 64, "consts2")
    o = 0
    ropep = Rc2.view(F32, [NTP, 24], o); o += NTP * 96
    ropeo = Rc2.view(F32, [NOWN, 24], o); o += NOWN * 96
    ropes = Rc2.view(F32, [24], o); o += 96
    gqr = Rc2.view(F32, [64], o); o += 256
    gkr = Rc2.view(F32, [64], o); o += 256
    brr = Rc2.view(F32, [36], o); o += 144
    wr_sb = Rc2.view(F32, [8, 36], o); o += 8 * 36 * 4
    n1g = Rc2.view(F32, [8], o); o += 32
    n2g = Rc2.view(F32, [8], o); o += 32
    cTs = Rc2.view(F32, [16], o); o += 64
    Bc2 = Rc2.b
    S.dma("sp", ropep, c_ropep.rearrange("p (a b) -> p a b", a=NTP), wbuf=Bc2)
    S.dma("sp", ropeo, c_ropeo.rearrange("p (a b) -> p a b", a=NOWN), wbuf=Bc2, more=True)
    S.dma("sp", ropes, c_ropes[:, :], wbuf=Bc2, more=True)
    S.dma("sp", gqr, gq[0:1, :].to_broadcast([128, 64]), wbuf=Bc2, more=True)
    S.dma("sp", gkr, gk[0:1, :].to_broadcast([128, 64]), wbuf=Bc2, more=True)
    S.dma("sp", brr, b_r[0:1, :].to_broadcast([128, 36]), wbuf=Bc2, more=True)
    S.dma("sp", wr_sb, w_r_v, wbuf=Bc2, more=True)
    S.dma("sp", n1g, n1gT[:, :], wbuf=Bc2, more=True)
    S.dma("sp", n2g, n2gT[:, :], wbuf=Bc2, more=True)
    S.dma("sp", cTs, cT[:, :], wbuf=Bc2, more=True)

    Rs = A(4096, "small")
    _so = [0]
    smallbufs = {}

    def small(name, n, dt=F32):
        esz = 2 if dt == BF16 else 4
        v = Rs.view(dt, [n], _so[0])
        _so[0] += (n * esz + 15) // 16 * 16
        smallbufs[name] = Rs.buf(name)
        return v, smallbufs[name]

    epsc, Beps = small("eps", 1)
    onef, Bonef = small("onef", 64)
    zc, Bzc = small("zc", 1)
    S.op("pool", lambda e: e.memset(epsc, 1e-6), writes=[Beps])
    S.op("pool", lambda e: e.memset(onef, 1.0), writes=[Bonef])
    S.op("pool", lambda e: e.memset(zc, 0.0), writes=[Bzc])
    ssq, Bssq = small("ssq", 1)
    lnv, Blnv = small("lnv", 1)
    rstd, Brstd = small("rstd", 1)
    ss8, Bss8 = small("ss8", 8)
    ln8, Bln8 = small("ln8", 8)
    rs8, Brs8 = small("rs8", 8)
    amax, Bamax = small("amax", 1)
    w0, Bw0 = small("w0", 1)
    Wt, BWt = small("Wt", 24)
    cc, Bcc = small("cc", 1)
    cnt, Bcnt = small("cnt", 1)
    tpp, Btpp = small("tpp", 1)
    ssum, Bssum = small("ssum", 1)
    tq, Btq = small("tq", 1)
    thr, Bthr = small("thr", 1)
    wi_sb, Bwi = small("wi", 4)
    modT, BmodT = small("modT", 96)
    A1, BA1 = small("A1", 16)
    B1, BB1 = small("B1", 16)
    A2, BA2 = small("A2", 16)
    B2, BB2 = small("B2", 16)
    tm16, Btm16 = small("tm16", 16)
    silc, Bsilc = small("silc", 16)

    GR = {}

    def phase0(do_G):
        Rslab = [A(8 * 512 * 4, f"adaslab{i}") for i in range(2)]
        Rscb = A(2 * 8 * 128 * 4, "scB"); scB = Rscb.view(F32, [2, 8, 128])
        Rbrow = [A(512 * 4, f"brow{i}") for i in range(2)]
        Rbt = A(48 * 4, "badaT"); bT = Rbt.view(F32, [48])
        S.dma("sp", bT, b_adaT[:, :], wbuf=Rbt.b)
        silv = silc.rearrange("p (k j) -> p k j", j=2)
        S.op("act", lambda e: e.activation(out=silc, in_=cTs, func=AF.Silu), reads=[Bc2], writes=[Bsilc])
        for j in range(2):
            S.op("dve", lambda e, j=j: e.tensor_copy(out=scB[:, j, :, :], in_=silv[:, :, j:j + 1].to_broadcast([128, 8, 128])),
                 reads=[Bsilc], writes=[Rscb.b])
        pm = bankf(0)
        first = True
        if do_G:
            GR["RG1"] = A(2 * 1024 * 4, "G1"); GR["RG2"] = A(2 * 1024 * 4, "G2")
            G1 = GR["RG1"].view(F32, [2, 1024]); G2 = GR["RG2"].view(F32, [2, 1024])
            RG1 = GR["RG1"]; RG2 = GR["RG2"]
        for s in range(12):
            seg = s // 2
            if do_G != (seg >= 2):
                continue
            R = Rslab[s % 2]
            sl = R.view(F32, [8, 512])
            S.dma("sp" if s % 2 == 0 else "pool", sl, w_ada_v[:, :, s * 512:(s + 1) * 512], wbuf=R.b)
            if seg in (2, 5):
                Rb = Rbrow[s % 2]
                S.dma("sp", Rb.view(F32, [512]), b_ada[0:1, s * 512:(s + 1) * 512].to_broadcast([128, 512]), wbuf=Rb.b)
                G = G1 if seg == 2 else G2
                RG = RG1 if seg == 2 else RG2
                half = s % 2
                for j in range(2):
                    bk = 1 + j
                    for k in range(8):
                        S.op("pe", lambda e, j=j, k=k, bk=bk: e.matmul(bankf(bk), scB[:, j, k, :], sl[:, k, :], start=(k == 0), stop=(k == 7)),
                             reads=[Rscb.b, R.b], writes=[Bbank[bk]])
                    S.op("dve", lambda e, j=j, bk=bk: e.tensor_tensor(out=G[:, j, half * 512:(half + 1) * 512], in0=bankf(bk), in1=Rb.view(F32, [512]), op=ALU.add),
                         reads=[Bbank[bk], Rb.b], writes=[RG.b])
            else:
                for nb in range(4):
                    blk = s * 4 + nb
                    for k in range(8):
                        S.op("pe", lambda e, nb=nb, k=k, blk=blk, first=first: e.matmul(pm[:, blk * 2:blk * 2 + 2], sl[:, k, nb * 128:(nb + 1) * 128], silv[:, k, :],
                                                                                         start=first, stop=(k == 7), skip_group_check=True),
                             reads=[R.b, Bsilc], writes=[Bbank[0]])
                        first = False
        mv = modT.rearrange("p (n j) -> p n j", j=2)
        pmv = pm[:, 0:96].rearrange("p (n j) -> p n j", j=2)
        for (a, b) in (((24, 40),) if do_G else ((0, 16),)):
            S.op("dve", lambda e, a=a, b=b: e.tensor_tensor(out=mv[:, a:b, :], in0=pmv[:, a:b, :], in1=bT[:, a:b].unsqueeze(2).to_broadcast([128, b - a, 2]), op=ALU.add),
                 reads=[Bbank[0], Rbt.b], writes=[BmodT])
        for (Ax, BAx, Bx, BBx, gT, so, sh) in (((A2, BA2, B2, BB2, n2g, 32, 24),) if do_G else ((A1, BA1, B1, BB1, n1g, 8, 0),)):
            Av = Ax.rearrange("p (k j) -> p k j", j=2)
            Bv = Bx.rearrange("p (k j) -> p k j", j=2)
            t16 = tm16.rearrange("p (k j) -> p k j", j=2)
            S.op("dve", lambda e, so=so: e.tensor_scalar(out=t16, in0=mv[:, so:so + 8, :], scalar1=1.0, scalar2=None, op0=ALU.add), reads=[BmodT], writes=[Btm16])
            S.op("dve", lambda e, Av=Av, gT=gT: e.tensor_tensor(out=Av, in0=t16, in1=gT.unsqueeze(2).to_broadcast([128, 8, 2]), op=ALU.mult), reads=[Btm16, Bc2], writes=[BAx])
            S.op("dve", lambda e, Bv=Bv, sh=sh: e.tensor_copy(out=Bv, in_=mv[:, sh:sh + 8, :]), reads=[BmodT], writes=[BBx])
        for r in Rslab + [Rscb] + Rbrow + [Rbt]:
            AR.release(r)

    try:
        phase0(False)
        A1v = A1.rearrange("p (k j) -> p k j", j=2); B1v = B1.rearrange("p (k j) -> p k j", j=2)
        A2v = A2.rearrange("p (k j) -> p k j", j=2); B2v = B2.rearrange("p (k j) -> p k j", j=2)

        Rxs = [A(1024 * 4, f"xs{i}") for i in range(2)]
        Rxn = A(1024 * 2, "xn")
        RhT = [A(8 * 128 * 2, f"hT{i}") for i in range(2)]
        Rst = {n: A(512 * 4, "st_" + n) for n in ("a", "b", "c")}
        Rtmp = A(512 * 4, "tmpf")
        Rbf = A(512 * 2, "bfc")
        Rbf2 = A(512 * 2, "bfc2")
        Rt1 = A(8 * 16 * 4, "t1")
        xn_b = Rxn.view(BF16, [1024])
        junk = Rtmp.view(BF16, [1024])
        XF = {}

        def norm_tile(xs_ap, Bx, rows, Av, Bv, j, hT_ap, BhT, tpbank, tpbank2, fp32=False, junkr=None):
            jv = junk if junkr is None else junkr.view(BF16, [1024])
            Bjk = Rtmp.b if junkr is None else junkr.b
            S.op("act", lambda e: e.activation(out=jv[:rows, :], in_=xs_ap, func=AF.Square, accum_out=ssq[:rows, :]), reads=[Bx], writes=[Bjk, Bssq])
            S.op("act", lambda e: e.activation(out=lnv[:rows, :], in_=ssq[:rows, :], func=AF.Ln, scale=1.0 / D, bias=epsc[:rows, :]), reads=[Bssq, Beps], writes=[Blnv])
            S.op("act", lambda e: e.activation(out=rstd[:rows, :], in_=lnv[:rows, :], func=AF.Exp, scale=-0.5), reads=[Blnv], writes=[Brstd])
            xn = XF["v"] if fp32 else xn_b
            Bxn = XF["b"] if fp32 else Rxn.b
            S.op("dve", lambda e: e.tensor_scalar(out=xn[:rows, :], in0=xs_ap, scalar1=rstd[:rows, :], scalar2=None, op0=ALU.mult), reads=[Bx, Brstd], writes=[Bxn])
            def tpv(k):
                bk = tpbank if k < 4 else tpbank2
                if fp32:
                    return bk, bankf(bk)[:, (k % 4) * 128:(k % 4) * 128 + rows]
                return bk, bankb(bk)[:, (k % 4) * 128:(k % 4) * 128 + rows]
            idn = identf if fp32 else identb
            for k in range(8):
                bk, tp = tpv(k)
                S.op("pe", lambda e, tp=tp, k=k: e.transpose(tp, xn[:rows, k * 128:(k + 1) * 128], idn[:rows, :rows]), reads=[Bxn, Bc], writes=[Bbank[bk]])
            for kk in range(8):
                k = (kk // 2) + 4 * (kk % 2)
                bk, tp = tpv(k)
                if k < 4:
                    S.op("dve", lambda e, tp=tp, k=k: e.tensor_scalar(out=hT_ap[:, k, :rows], in0=tp, scalar1=Av[:, k, j:j + 1], scalar2=Bv[:, k, j:j + 1], op0=ALU.mult, op1=ALU.add),
                         reads=[Bbank[bk], BA1, BB1, BA2, BB2], writes=[BhT.buf(k)])
                else:
                    S.op("act", lambda e, tp=tp, k=k: e.activation(out=hT_ap[:, k, :rows], in_=tp, func=AF.Identity, scale=Av[:, k, j:j + 1], bias=Bv[:, k, j:j + 1]),
                         reads=[Bbank[bk], BA1, BB1, BA2, BB2], writes=[BhT.buf(k)])

        def proj(hT_ap, BhT, rows, slab, Bslab, c0, n, bk):
            for k in range(8):
                S.op("pe", lambda e, k=k: e.matmul(bankf(bk)[:rows, 0:n], hT_ap[:, k, :rows], slab[:, k, c0:c0 + n], start=(k == 0), stop=(k == 7)),
                     reads=[BhT.buf(k), Bslab], writes=[Bbank[bk]])

        def headnorm(st, Bst, rows, grow, nheads=8):
            tmp = Rtmp.view(F32, [512])
            n = nheads * 64
            S.op("dve", lambda e: e.tensor_tensor(out=tmp[:rows, :n], in0=st[:rows, :n], in1=st[:rows, :n], op=ALU.mult), reads=[Bst], writes=[Rtmp.b])
            S.op("dve", lambda e: e.reduce_sum(out=ss8[:rows, :nheads], in_=tmp[:rows, :n].rearrange("p (h d) -> p h d", d=64), axis=AX.X), reads=[Rtmp.b], writes=[Bss8])
            S.op("act", lambda e: e.activation(out=ln8[:rows, :nheads], in_=ss8[:rows, :nheads], func=AF.Ln, scale=1.0 / 64, bias=epsc[:rows, :]), reads=[Bss8, Beps], writes=[Bln8])
            S.op("act", lambda e: e.activation(out=rs8[:rows, :nheads], in_=ln8[:rows, :nheads], func=AF.Exp, scale=-0.5), reads=[Bln8], writes=[Brs8])
            sv = st[:rows, :n].rearrange("p (h d) -> p h d", d=64)
            S.op("dve", lambda e: e.tensor_tensor(out=sv, in0=sv, in1=rs8[:rows, :nheads].unsqueeze(2).to_broadcast([rows, nheads, 64]), op=ALU.mult), reads=[Bst, Brs8], writes=[Bst])
            S.op("dve", lambda e: e.tensor_tensor(out=sv, in0=sv, in1=grow[:rows, :].unsqueeze(1).to_broadcast([rows, nheads, 64]), op=ALU.mult), reads=[Bst, Bc2], writes=[Bst])

        def rope(st, Bst, rows, nheads, rt):
            sv = st[:rows, :nheads * 64].rearrange("p (h d) -> p h d", d=64)
            t1 = Rt1.view(F32, [8, 16])
            sinb = rt[:rows, 16:24].unsqueeze(1).to_broadcast([rows, nheads, 8])
            cosb = rt[:rows, 0:16].unsqueeze(1).to_broadcast([rows, nheads, 16])
            S.op("pool", lambda e: e.tensor_tensor(out=t1[:rows, :nheads, 0:8], in0=sv[:, :, 8:16], in1=sinb, op=ALU.mult), reads=[Bst, Bc2], writes=[Rt1.b])
            S.op("pool", lambda e: e.tensor_tensor(out=t1[:rows, :nheads, 8:16], in0=sv[:, :, 0:8], in1=sinb, op=ALU.mult), reads=[Bst, Bc2], writes=[Rt1.b])
            S.op("pool", lambda e: e.tensor_tensor(out=sv[:, :, 0:16], in0=sv[:, :, 0:16], in1=cosb, op=ALU.mult), reads=[Bst, Bc2, Rt1.b], writes=[Bst])
            S.op("pool", lambda e: e.tensor_tensor(out=sv[:, :, 0:8], in0=sv[:, :, 0:8], in1=t1[:rows, :nheads, 0:8], op=ALU.subtract), reads=[Bst, Rt1.b], writes=[Bst])
            S.op("pool", lambda e: e.tensor_tensor(out=sv[:, :, 8:16], in0=sv[:, :, 8:16], in1=t1[:rows, :nheads, 8:16], op=ALU.add), reads=[Bst, Rt1.b], writes=[Bst])

        def to_T(src_bf, Bsrc, rows, npair, dstT, BdstT, col0, bk, eng="act", zpad=False):
            tp = bankb(bk)
            for p_ in range(npair):
                S.op("pe", lambda e, p_=p_: e.transpose(tp[:, p_ * 128:p_ * 128 + rows], src_bf[:rows, p_ * 128:(p_ + 1) * 128], identb[:rows, :rows]), reads=[Bsrc, Bc], writes=[Bbank[bk]])
            src = tp[:, 0:npair * 128].rearrange("p (a b) -> p a b", a=npair)[:, :, :rows]
            if zpad:
                for hh in range(2):
                    lo = hh * 64
                    if eng == "act":
                        S.op("act", lambda e: e.activation(out=dstT[lo:lo + 64, :, hh, col0:col0 + rows], in_=src[lo:lo + 64], func=AF.Copy), reads=[Bbank[bk]], writes=[BdstT])
                    else:
                        S.op("dve", lambda e: e.tensor_copy(out=dstT[lo:lo + 64, :, hh, col0:col0 + rows], in_=src[lo:lo + 64]), reads=[Bbank[bk]], writes=[BdstT])
                return
            if eng == "act":
                S.op("act", lambda e: e.activation(out=dstT[:, :, col0:col0 + rows], in_=src, func=AF.Copy), reads=[Bbank[bk]], writes=[BdstT])
            else:
                S.op("dve", lambda e: e.tensor_copy(out=dstT[:, :, col0:col0 + rows], in_=src), reads=[Bbank[bk]], writes=[BdstT])

        RoT = A(8 * TOWN * 2, "oT"); oT = RoT.view(BF16, [8, TOWN])

        def mixer_A(group):
            prompt = group == "p"
            NT = NTP if prompt else NTS
            NKT = NT * 128
            jmod = 0 if prompt else 1
            RkaT = A(4 * NKT * 2, "kaT"); kaT = RkaT.view(BF16, [4, NKT])
            RkiT = A(NKT * 2, "kiT"); kiT = RkiT.view(BF16, [NKT])
            Rva = A(NT * 520 * 2, "va"); va = Rva.view(BF16, [NT, 8, 65])
            S.op("pool", lambda e: e.memset(va[:, :, :, 64:65], 1.0), writes=[Rva.b])
            if not prompt:
                S.op("pool", lambda e: e.memset(va[64:128, NT - 1, :, :], 0.0), writes=[Rva.b])
                S.op("pool", lambda e: e.memset(kaT[:, :, NKT - 64:NKT], 0.0), writes=[RkaT.b])
                S.op("pool", lambda e: e.memset(kiT[:, NKT - 64:NKT], 0.0), writes=[RkiT.b])
            Rslab = A(8 * 1088 * 2, "KAslab"); slab = Rslab.view(BF16, [8, 1088])
            S.dma("pool", slab[:, :, 0:1024], w_in_v[:, :, C_KA:C_KA + 1024], wbuf=Rslab.b)
            S.dma("pool", slab[:, :, 1024:1088], w_in_v[:, :, C_KI:C_KI + 64], wbuf=Rslab.b, more=True)
            Rst2 = {n: A(512 * 4, "st2_" + n) for n in ("a", "b", "c", "d", "e", "f")}
            Rjk = A(1024 * 2, "kjunk")
            STK = [(Rst["a"], Rst["b"], Rst["c"]), (Rst2["a"], Rst2["b"], Rst2["c"]), (Rst2["d"], Rst2["e"], Rst2["f"])]
            kbf = Rbf.view(BF16, [512]); ibf = Rbf2.view(BF16, [512])

            def kfront(t):
                Ra, Rb_, Rc_ = STK[t % 3]
                st_k = Ra.view(F32, [512]); st_v = Rb_.view(F32, [512]); st_i = Rc_.view(F32, [512])
                Bsk, Bsv, Bsi = Ra.b, Rb_.b, Rc_.b
                rows = 128 if (prompt or t < NT - 1) else 64
                from_cache = (not prompt) and t < NT - 1
                if from_cache:
                    S.dma("sp", st_k, ca_k[t * 128:(t + 1) * 128, :], wbuf=Bsk)
                    S.dma("sp", st_v, ca_v[t * 128:(t + 1) * 128, :], wbuf=Bsv)
                    S.dma("sp", st_i[:, 0:64], ca_ki[t * 128:(t + 1) * 128, :], wbuf=Bsi)
                else:
                    Rx = Rxs[t % 2]; xs = Rx.view(F32, [1024])
                    src = x_p[t * 128:(t + 1) * 128, :] if prompt else x_s[:, :]
                    S.dma("sp", xs[:rows, :], src, wbuf=Rx.b)
                    RH = RhT[t % 2]; hT = RH.view(BF16, [8, 128])
                    norm_tile(xs[:rows, :], Rx.b, rows, A1v, B1v, jmod, hT, RH, 0, 6, junkr=Rjk)
                    proj(hT, RH, rows, slab, Rslab.b, 0, 512, 1)
                    proj(hT, RH, rows, slab, Rslab.b, 512, 512, 2)
                    proj(hT, RH, rows, slab, Rslab.b, 1024, 64, 3)
                    S.op("act", lambda e: e.activation(out=st_k[:rows, :], in_=bankf(1)[:rows, :], func=AF.Copy), reads=[Bbank[1]], writes=[Bsk])
                    S.op("dve", lambda e: e.tensor_copy(out=st_v[:rows, :], in_=bankf(2)[:rows, :]), reads=[Bbank[2]], writes=[Bsv])
                    S.op("dve", lambda e: e.tensor_copy(out=st_i[:rows, 0:64], in_=bankf(3)[:rows, 0:64]), reads=[Bbank[3]], writes=[Bsi])

            def kback1(t):
                Ra, Rb_, Rc_ = STK[t % 3]
                st_k = Ra.view(F32, [512]); st_v = Rb_.view(F32, [512]); st_i = Rc_.view(F32, [512])
                Bsk, Bsv, Bsi = Ra.b, Rb_.b, Rc_.b
                rows = 128 if (prompt or t < NT - 1) else 64
                from_cache = (not prompt) and t < NT - 1
                if not from_cache:
                    rt = ropep[:, t, :] if prompt else ropes
                    headnorm(st_k, Bsk, rows, gkr)
                    rope(st_k, Bsk, rows, 8, rt)
                    rope(st_i, Bsi, rows, 1, rt)
                    if prompt:
                        S.dma("pool", o_nak[t * 128:(t + 1) * 128, :], st_k, rbuf=Bsk, is_output=True)
                        S.dma("pool", o_nav[t * 128:(t + 1) * 128, :], st_v, rbuf=Bsv, is_output=True)
                        S.dma("pool", o_naki[t * 128:(t + 1) * 128, :], st_i[:, 0:64], rbuf=Bsi, is_output=True)
                    else:
                        S.dma("pool", s_nak[:, :], st_k[:64, :], rbuf=Bsk, is_output=True)
                        S.dma("pool", s_nav[:, :], st_v[:64, :], rbuf=Bsv, is_output=True)
                        S.dma("pool", s_naki[:, :], st_i[:64, 0:64], rbuf=Bsi, is_output=True)

            def kback2(t):
                Ra, Rb_, Rc_ = STK[t % 3]
                st_k = Ra.view(F32, [512]); st_v = Rb_.view(F32, [512]); st_i = Rc_.view(F32, [512])
                Bsk, Bsv, Bsi = Ra.b, Rb_.b, Rc_.b
                rows = 128 if (prompt or t < NT - 1) else 64
                S.op("act", lambda e: e.activation(out=kbf[:rows, :], in_=st_k[:rows, :], func=AF.Copy), reads=[Bsk], writes=[Rbf.b])
                to_T(kbf, Rbf.b, rows, 4, kaT, RkaT.b, t * 128, 4, eng="act")
                S.op("dve", lambda e: e.tensor_copy(out=va[:rows, t, :, 0:64], in_=st_v[:rows, :].rearrange("p (h d) -> p h d", d=64)), reads=[Bsv], writes=[Rva.b])
                S.op("dve", lambda e: e.tensor_copy(out=ibf[:rows, 0:128].rearrange("p (a d) -> p a d", a=2), in_=st_i[:rows, 0:64].unsqueeze(1).to_broadcast([rows, 2, 64])), reads=[Bsi], writes=[Rbf2.b])
                tp = bankb(5)
                S.op("pe", lambda e: e.transpose(tp[:, 0:rows], ibf[:rows, 0:128], identb[:rows, :rows]), reads=[Rbf2.b, Bc], writes=[Bbank[5]])
                S.op("dve", lambda e: e.tensor_copy(out=kiT[:, t * 128:t * 128 + rows], in_=tp[:, 0:rows]), reads=[Bbank[5]], writes=[RkiT.b])

            for step in range(NT + 2):
                if step < NT:
                    kfront(step)
                if 1 <= step <= NT:
                    kback1(step - 1)
                if step >= 2:
                    kback2(step - 2)
            for r_ in list(Rst2.values()) + [Rjk]:
                AR.release(r_)
            AR.release(Rslab)
            chk(0.5 if prompt else 1.5)

            Rqs = A(8 * 772 * 2, "QAslab"); qs = Rqs.view(BF16, [8, 772])
            S.dma("pool", qs[:, :, 0:512], w_in_v[:, :, C_QA:C_QA + 512], wbuf=Rqs.b)
            S.dma("pool", qs[:, :, 512:768], w_in_v[:, :, C_QI:C_QI + 256], wbuf=Rqs.b, more=True)
            S.dma("pool", qs[:, :, 768:772], w_in_v[:, :, C_WI:C_WI + 4], wbuf=Rqs.b, more=True)
            Rsc = A(NKT * 4, "score"); score = Rsc.view(F32, [NKT])
            nb2 = 2 if prompt else 1
            Rmb = [A(NKT * 2, f"mb{i}") for i in range(nb2)] * (3 - nb2)
            Rrt = [A(512 * 4, f"rtmp{i}") for i in range(2)]
            RPt = [A(512 * 2, f"Pt{i}") for i in range(2)]
            RqaT = [A(8 * 128 * 2, f"qaT{i}") for i in range(2)]
            RqiT = [A(4 * 128 * 2, f"qiT{i}") for i in range(2)]
            for r_ in RqaT + RqiT:
                S.op("pool", lambda e, r_=r_: e.memset(r_.view(BF16, [r_.size // 2]), 0.0), writes=[r_.b])
            Rbcs = A(512 * 4, "bcs"); bcs = Rbcs.view(F32, [512])
            rden = bcs
            Brd = Rbcs.buf("rd"); Bbcs = Rbcs.buf("bc")
            st_q = Rst["a"].view(F32, [512]); st_qi = Rst["b"].view(F32, [512])
            Bsq, Bsqi = Rst["a"].b, Rst["b"].b
            qbf = Rbf.view(BF16, [512]); qibf = Rbf2.view(BF16, [512])
            ntiles = NOWN if prompt else 1
            ucount = [0]

            def qside(m):
                rows = 128 if prompt else 64
                Rx = Rxs[m % 2]; xs = Rx.view(F32, [1024])
                src = x_own[m * 128:(m + 1) * 128, :] if prompt else x_s[:, :]
                S.dma("sp", xs[:rows, :], src, wbuf=Rx.b)
                RH = RhT[m % 2]; hT = RH.view(BF16, [8, 128])
                norm_tile(xs[:rows, :], Rx.b, rows, A1v, B1v, jmod, hT, RH, 0, 7)
                proj(hT, RH, rows, qs, Rqs.b, 0, 512, 1)
                proj(hT, RH, rows, qs, Rqs.b, 512, 260, 0)
                S.op("act", lambda e: e.activation(out=st_q[:rows, :], in_=bankf(1)[:rows, :], func=AF.Copy), reads=[Bbank[1]], writes=[Bsq])
                S.op("dve", lambda e: e.tensor_copy(out=st_qi[:rows, 0:260], in_=bankf(0)[:rows, 0:260]), reads=[Bbank[0]], writes=[Bsqi])
                rt = ropeo[:, m, :] if prompt else ropes
                headnorm(st_q, Bsq, rows, gqr)
                rope(st_q, Bsq, rows, 8, rt)
                rope(st_qi, Bsqi, rows, 4, rt)
                S.op("pool", lambda e: e.tensor_scalar(out=qbf[:rows, :], in0=st_q[:rows, :], scalar1=0.125, scalar2=None, op0=ALU.mult), reads=[Bsq], writes=[Rbf.b])
                S.op("pool", lambda e: e.tensor_scalar(out=qibf[:rows, 0:256], in0=st_qi[:rows, 0:256], scalar1=0.125, scalar2=None, op0=ALU.mult), reads=[Bsqi], writes=[Rbf2.b])
                S.op("dve", lambda e: e.tensor_copy(out=wi_sb[:rows, :], in_=st_qi[:rows, 256:260]), reads=[Bsqi], writes=[Bwi])
                Rq = RqaT[m % 2]; Ri = RqiT[m % 2]
                to_T(qbf, Rbf.b, rows, 4, Rq.view(BF16, [4, 2, 128]), Rq.b, 0, 1, eng="act", zpad=True)
                to_T(qibf, Rbf2.b, rows, 2, Ri.view(BF16, [2, 2, 128]), Ri.b, 0, 0, eng="dve", zpad=True)

            def scores(m):
                rows = 128 if prompt else 64
                j = m // 2
                NK = 512 * (j + 1) if prompt else NKT
                qiT = RqiT[m % 2].view(BF16, [2, 2, 128]); BqiT = RqiT[m % 2].b
                mb = Rmb[m % 2].view(BF16, [NKT]); Bmb = Rmb[m % 2].b
                nch = (NK + 511) // 512
                for c in range(nch):
                    n = min(512, NK - c * 512)
                    for h in range(4):
                        bk = h % 2
                        S.op("pe", lambda e, h=h, c=c, n=n, bk=bk: e.matmul(bankf(bk)[:rows, 0:n], qiT[:, h // 2, h % 2, :rows], kiT[:, c * 512:c * 512 + n], start=True, stop=True),
                             reads=[BqiT, RkiT.b], writes=[Bbank[bk]])
                        sc = score[:rows, c * 512:c * 512 + n]
                        if h == 0:
                            S.op("dve", lambda e, sc=sc, n=n, bk=bk: e.tensor_scalar(out=sc, in0=bankf(bk)[:rows, 0:n], scalar1=0.0, scalar2=wi_sb[:rows, 0:1], op0=ALU.max, op1=ALU.mult),
                                 reads=[Bbank[bk], Bwi], writes=[Rsc.b])
                        else:
                            Rr = Rrt[h % 2]; rt_ = Rr.view(F32, [512])
                            S.op("act", lambda e, n=n, bk=bk, rt_=rt_: e.activation(out=rt_[:rows, 0:n], in_=bankf(bk)[:rows, 0:n], func=AF.Relu), reads=[Bbank[bk]], writes=[Rr.b])
                            S.op("dve", lambda e, sc=sc, n=n, h=h, rt_=rt_: e.scalar_tensor_tensor(out=sc, in0=rt_[:rows, 0:n], scalar=wi_sb[:rows, h:h + 1], in1=sc, op0=ALU.mult, op1=ALU.add),
                                 reads=[Rr.b, Bwi, Rsc.b], writes=[Rsc.b])
                S.op("dve", lambda e: e.reduce_max(out=amax[:rows, :], in_=score[:rows, 0:NK], axis=AX.X, apply_absolute_value=True), reads=[Rsc.b], writes=[Bamax])
                S.op("dve", lambda e: e.tensor_scalar(out=w0[:rows, :], in0=amax[:rows, :], scalar1=1.0, scalar2=None, op0=ALU.add), reads=[Bamax], writes=[Bw0])
                S.op("dve", lambda e: e.tensor_scalar(out=Wt[:rows, :], in0=pow2[:rows, :], scalar1=w0[:rows, :], scalar2=None, op0=ALU.mult), reads=[Bw0, Bc], writes=[BWt])
                if prompt:
                    S.op("dve", lambda e: e.tensor_tensor(out=score[:rows, NK - 512:NK], in0=score[:rows, NK - 512:NK], in1=adm[:rows, m % 2, :], op=ALU.add), reads=[Rsc.b, Bc], writes=[Rsc.b])
                else:
                    S.op("dve", lambda e: e.memset(score[:rows, NK - 64:NK], -1e30), writes=[Rsc.b])
                S.op("dve", lambda e: e.memset(cc[:rows, :], 0.0), writes=[Bcc])

            def bis_params(m):
                rows = 128 if prompt else 64
                NK = 512 * (m // 2 + 1) if prompt else NKT
                a = max(64, int(0.42 * NK) // 64 * 64)
                return rows, NK, a

            def bis_iter(m, it):
                rows, NK, a = bis_params(m)
                mb = Rmb[m % 2].view(BF16, [NKT])
                Blo = Rmb[m % 2].buf("lo"); Bhi = Rmb[m % 2].buf("hi")
                S.op("dve", lambda e: e.tensor_scalar(out=mb[:rows, 0:NK], in0=score[:rows, 0:NK], scalar1=cc[:rows, :], scalar2=None, op0=ALU.is_ge, op1=ALU.add, accum_out=cnt[:rows, :]),
                     reads=[Rsc.b, Bcc], writes=[Blo, Bhi, Bcnt])
                S.op("dve", lambda e: e.tensor_scalar(out=tpp[:rows, :], in0=cnt[:rows, :], scalar1=255.5, scalar2=0.5, op0=ALU.is_ge, op1=ALU.subtract), reads=[Bcnt], writes=[Btpp])
                S.op("dve", lambda e: e.scalar_tensor_tensor(out=cc[:rows, :], in0=tpp[:rows, :], scalar=Wt[:rows, it:it + 1], in1=cc[:rows, :], op0=ALU.mult, op1=ALU.add),
                     reads=[Btpp, BWt, Bcc], writes=[Bcc])

            def bis_final(m):
                rows, NK, a = bis_params(m)
                mb = Rmb[m % 2].view(BF16, [NKT])
                Blo = Rmb[m % 2].buf("lo"); Bhi = Rmb[m % 2].buf("hi")
                S.op("dve", lambda e: e.tensor_tensor(out=thr[:rows, :], in0=cc[:rows, :], in1=Wt[:rows, NIT:NIT + 1], op=ALU.subtract), reads=[Bcc, BWt], writes=[Bthr])
                S.op("dve", lambda e: e.tensor_scalar(out=mb[:rows, 0:NK], in0=score[:rows, 0:NK], scalar1=thr[:rows, :], scalar2=NEG, op0=ALU.is_lt, op1=ALU.mult),
                     reads=[Rsc.b, Bthr], writes=[Blo, Bhi])

            def attn(m, tick=None):
                rows = 128 if prompt else 64
                j = m // 2
                NKB = 4 * (j + 1) if prompt else NT
                HG = 512 // rows
                qaT = RqaT[m % 2].view(BF16, [4, 2, 128]); BqaT = RqaT[m % 2].b
                mb = Rmb[m % 2].view(BF16, [NKT])
                Blo = Rmb[m % 2].buf("lo"); Bhi = Rmb[m % 2].buf("hi")
                tok0 = m * 128 if prompt else NOWN * 128
                for g in range(8 // HG):
                    bo = 4 + (ucount[0] % 2)
                    ucount[0] += 1
                    Ov = bankf(bo)[:, 0:HG * rows].rearrange("p (a b) -> p a b", a=HG)
                    units = list(range(NKB))

                    def s_stage(kb, u):
                        bs = 2 + (u % 2)
                        for hs in range(HG):
                            h = g * HG + hs
                            pr, hh = h // 2, h % 2
                            S.op("pe", lambda e, hs=hs, pr=pr, hh=hh, kb=kb, bs=bs: e.matmul(bankf(bs)[:, hs * rows:(hs + 1) * rows], kaT[:, pr, kb * 128:(kb + 1) * 128], qaT[:, pr, hh, :rows], start=(hs == 0), stop=False, skip_group_check=True),
                                 reads=[RkaT.b, BqaT], writes=[Bbank[bs]])
                        for hs in range(HG):
                            S.op("pe", lambda e, hs=hs, kb=kb, bs=bs: e.matmul(bankf(bs)[:, hs * rows:(hs + 1) * rows], mb[:rows, kb * 128:(kb + 1) * 128], identb[:rows, :rows],
                                                                             start=False, stop=(hs == HG - 1), skip_group_check=True),
                                 reads=[Blo, Bhi, Bc], writes=[Bbank[bs]])

                    def p_stage(kb, u):
                        bs = 2 + (u % 2)
                        RP = RPt[u % 2]; Pt = RP.view(BF16, [512])
                        S.op("act", lambda e: e.activation(out=Pt[:, 0:HG * rows], in_=bankf(bs)[:, 0:HG * rows], func=AF.Exp), reads=[Bbank[bs]], writes=[RP.b])
                        for hs in range(HG):
                            h = g * HG + hs
                            S.op("pe", lambda e, hs=hs, h=h, kb=kb: e.matmul(bankf(bo)[0:65, hs * rows:(hs + 1) * rows], va[:, kb, h, :], Pt[:, hs * rows:(hs + 1) * rows],
                                                                           start=(kb == 0 and hs == 0), stop=(kb == NKB - 1), skip_group_check=True),
                                 reads=[Rva.b, RP.b], writes=[Bbank[bo]])

                    for u in range(NKB + 1):
                        if u < NKB:
                            s_stage(units[u], u)
                            chk(0.73)
                        if u >= 1:
                            p_stage(units[u - 1], u - 1)
                            chk(0.74)
                            if tick is not None:
                                tick()
                    chk(0.75)
                    n = HG * rows
                    S.op("act", lambda e: e.activation(out=rden[64:65, 0:n], in_=bankf(bo)[64:65, 0:n], func=AF.Ln), reads=[Bbank[bo]], writes=[Brd])
                    S.op("act", lambda e: e.activation(out=rden[64:65, 0:n], in_=rden[64:65, 0:n], func=AF.Exp, scale=-1.0), reads=[Brd], writes=[Brd])
                    S.op("pe", lambda e: e.matmul(bankf(6)[0:64, 0:n], onef[64:65, 0:64], rden[64:65, 0:n], start=True, stop=True), reads=[Bonef, Brd], writes=[Bbank[6]])
                    S.op("act", lambda e: e.activation(out=bcs[0:64, 0:n], in_=bankf(6)[0:64, 0:n], func=AF.Copy), reads=[Bbank[6]], writes=[Bbcs])
                    S.op("dve", lambda e, g=g: e.tensor_tensor(out=oT[0:64, g * HG:(g + 1) * HG, tok0:tok0 + rows], in0=Ov[0:64, :, :],
                                                              in1=bcs[0:64, 0:n].rearrange("p (a b) -> p a b", a=HG), op=ALU.mult),
                         reads=[Bbank[bo], Bbcs], writes=[RoT.buf("a")])

            qside(0)
            chk(0.6 if prompt else 1.6)
            scores(0)
            for it in range(NIT):
                bis_iter(0, it)
            bis_final(0)
            chk(0.7 if prompt else 1.7)
            for m in range(ntiles):
                pend = []
                if m + 1 < ntiles:
                    qside(m + 1)
                    scores(m + 1)
                    pend = list(range(NIT))
                nun = (8 // (512 // (128 if prompt else 64))) * (4 * (m // 2 + 1) if prompt else NT)
                state = {"done": 0, "units": 0}

                def tick(m=m, pend=pend, state=state, nun=nun):
                    state["units"] += 1
                    target = min(len(pend), (state["units"] * len(pend) + nun - 1) // nun)
                    while state["done"] < target:
                        bis_iter(m + 1, pend[state["done"]])
                        state["done"] += 1
                attn(m, tick)
                while state["done"] < len(pend):
                    bis_iter(m + 1, pend[state["done"]])
                    state["done"] += 1
                if m + 1 < ntiles:
                    bis_final(m + 1)
                chk(0.8 if prompt else 1.8)
            for r in [Rqs, Rsc] + Rmb[:nb2] + Rrt + RPt + RqaT + RqiT + [Rbcs, RkaT, RkiT, Rva]:
                AR.release(r)

        def mixer_B(group):
            prompt = group == "p"
            NT = NTP if prompt else NTS
            NKT = NT * 128
            jmod = 0 if prompt else 1
            RkbT = A(4 * NKT * 2, "kbT"); kbT = RkbT.view(BF16, [4, NKT])
            Rvb = A(NT * 512 * 2, "vb"); vb = Rvb.view(BF16, [NT, 512])
            if not prompt:
                S.op("pool", lambda e: e.memset(vb[64:128, NT - 1, :], 0.0), writes=[Rvb.b])
                S.op("pool", lambda e: e.memset(kbT[:, :, NKT - 64:NKT], 0.0), writes=[RkbT.b])
            Rslab = A(8 * 1024 * 2, "KBslab"); slab = Rslab.view(BF16, [8, 1024])
            S.dma("pool", slab, w_in_v[:, :, C_KB:C_KB + 1024], wbuf=Rslab.b)
            Rst2 = {n: A(512 * 4, "st2b_" + n) for n in ("a", "b", "c", "d")}
            Rjk = A(1024 * 2, "kjunkb")
            STK = [(Rst["a"], Rst["b"]), (Rst2["a"], Rst2["b"]), (Rst2["c"], Rst2["d"])]
            kbf = Rbf.view(BF16, [512])

            def kfront(t):
                Ra, Rb_ = STK[t % 3]
                st_k = Ra.view(F32, [512]); st_v = Rb_.view(F32, [512])
                Bsk, Bsv = Ra.b, Rb_.b
                rows = 128 if (prompt or t < NT - 1) else 64
                from_cache = (not prompt) and t < NT - 1
                if from_cache:
                    S.dma("sp", st_k, cb_k[t * 128:(t + 1) * 128, :], wbuf=Bsk)
                    S.dma("sp", st_v, cb_v[t * 128:(t + 1) * 128, :], wbuf=Bsv)
                else:
                    Rx = Rxs[t % 2]; xs = Rx.view(F32, [1024])
                    src = x_p[t * 128:(t + 1) * 128, :] if prompt else x_s[:, :]
                    S.dma("sp", xs[:rows, :], src, wbuf=Rx.b)
                    RH = RhT[t % 2]; hT = RH.view(BF16, [8, 128])
                    norm_tile(xs[:rows, :], Rx.b, rows, A1v, B1v, jmod, hT, RH, 0, 6, junkr=Rjk)
                    proj(hT, RH, rows, slab, Rslab.b, 0, 512, 1)
                    proj(hT, RH, rows, slab, Rslab.b, 512, 512, 2)
                    S.op("act", lambda e: e.activation(out=st_k[:rows, :], in_=bankf(1)[:rows, :], func=AF.Copy), reads=[Bbank[1]], writes=[Bsk])
                    S.op("dve", lambda e: e.tensor_copy(out=st_v[:rows, :], in_=bankf(2)[:rows, :]), reads=[Bbank[2]], writes=[Bsv])

            def kback(t):
                Ra, Rb_ = STK[t % 3]
                st_k = Ra.view(F32, [512]); st_v = Rb_.view(F32, [512])
                Bsk, Bsv = Ra.b, Rb_.b
                rows = 128 if (prompt or t < NT - 1) else 64
                from_cache = (not prompt) and t < NT - 1
                if not from_cache:
                    if prompt:
                        S.dma("pool", o_nbk[t * 128:(t + 1) * 128, :], st_k, rbuf=Bsk, is_output=True)
                        S.dma("pool", o_nbv[t * 128:(t + 1) * 128, :], st_v, rbuf=Bsv, is_output=True)
                    else:
                        S.dma("pool", s_nbk[:, :], st_k[:64, :], rbuf=Bsk, is_output=True)
                        S.dma("pool", s_nbv[:, :], st_v[:64, :], rbuf=Bsv, is_output=True)
                S.op("act", lambda e: e.activation(out=kbf[:rows, :], in_=st_k[:rows, :], func=AF.Copy), reads=[Bsk], writes=[Rbf.b])
                to_T(kbf, Rbf.b, rows, 4, kbT, RkbT.b, t * 128, 4, eng="act")
                S.op("dve", lambda e: e.tensor_copy(out=vb[:rows, t, :], in_=st_v[:rows, :]), reads=[Bsv], writes=[Rvb.b])

            for step in range(NT + 2):
                if step < NT:
                    kfront(step)
                if step >= 2:
                    kback(step - 2)
            for r_ in list(Rst2.values()) + [Rjk]:
                AR.release(r_)
            AR.release(Rslab)

            Rqs = A(8 * 512 * 2, "QBslab"); qs = Rqs.view(BF16, [8, 512])
            S.dma("pool", qs, w_in_v[:, :, C_QB:C_QB + 512], wbuf=Rqs.b)
            nq = 256 if prompt else 64
            HG = 512 // nq
            RqbT = [A(8 * 256 * 2, f"qbT{i}") for i in range(2)]
            for r_ in RqbT:
                S.op("pool", lambda e, r_=r_: e.memset(r_.view(BF16, [r_.size // 2]), 0.0), writes=[r_.b])
            Re = [A(512 * 4, f"e{i}") for i in range(2)]
            Rsp = [A(512 * 2, f"sp{i}") for i in range(2)]
            RPt = [A(512 * 2, f"PtB{i}") for i in range(2)]
            RSl = [A(512 * 2, f"Sl{i}") for i in range(2)]
            st_q = Rst["c"].view(F32, [512]); Bsq = Rst["c"].b
            qbf = Rbf2.view(BF16, [512])
            nblk = NOWN // 2 if prompt else 1
            gcount = [0]

            def qside(j):
                Rq = RqbT[j % 2]; qbT = Rq.view(BF16, [4, 2, 256])
                for w in range(2 if prompt else 1):
                    m = 2 * j + w
                    rows = 128 if prompt else 64
                    Rx = Rxs[m % 2]; xs = Rx.view(F32, [1024])
                    src = x_own[m * 128:(m + 1) * 128, :] if prompt else x_s[:, :]
                    S.dma("sp", xs[:rows, :], src, wbuf=Rx.b)
                    RH = RhT[m % 2]; hT = RH.view(BF16, [8, 128])
                    norm_tile(xs[:rows, :], Rx.b, rows, A1v, B1v, jmod, hT, RH, 4, 5)
                    proj(hT, RH, rows, qs, Rqs.b, 0, 512, 1)
                    S.op("act", lambda e: e.activation(out=st_q[:rows, :], in_=bankf(1)[:rows, :], func=AF.Copy), reads=[Bbank[1]], writes=[Bsq])
                    S.op("pool", lambda e: e.tensor_scalar(out=qbf[:rows, :], in0=st_q[:rows, :], scalar1=0.125, scalar2=None, op0=ALU.mult), reads=[Bsq], writes=[Rbf2.b])
                    to_T(qbf, Rbf2.b, rows, 4, qbT, Rq.b, w * 128, 1, eng="dve", zpad=True)

            def attn(j):
                Rq = RqbT[j % 2]; qbT = Rq.view(BF16, [4, 2, 256])
                NKB = 4 * (j + 1) if prompt else NT
                ND = 4 if prompt else 1
                tok0 = j * 256 if prompt else NOWN * 128
                n = HG * nq
                for g in range(8 // HG):
                    gi = gcount[0]
                    gcount[0] += 1
                    bo = 6 + (gi % 2)
                    RS = RSl[gi % 2]; Sl = RS.view(BF16, [512])
                    S.op("pool", lambda e: e.memset(Sl[:, 0:n], 0.0), writes=[RS.b])
                    order = list(range(NKB - 1, -1, -1))

                    def zmm(bk_, kb, diag, b):
                        for hs in range(HG):
                            h = g * HG + hs
                            pr, hh = h // 2, h % 2
                            S.op("pe", lambda e, hs=hs, pr=pr, hh=hh: e.matmul(bankf(bk_)[:, hs * nq:(hs + 1) * nq], kbT[:, pr, kb * 128:(kb + 1) * 128], qbT[:, pr, hh, 0:nq], start=(hs == 0), stop=False, skip_group_check=True),
                                 reads=[RkbT.b, Rq.b], writes=[Bbank[bk_]])
                        if diag:
                            for hs in range(HG):
                                if prompt:
                                    for w in range(2):
                                        S.op("pe", lambda e, hs=hs, w=w: e.matmul(bankf(bk_)[:, hs * nq + w * 128:hs * nq + (w + 1) * 128], sbm[:, b, w, :], identb[:, :],
                                                                                start=False, stop=False, skip_group_check=True),
                                             reads=[Bc], writes=[Bbank[bk_]])
                                else:
                                    S.op("pe", lambda e, hs=hs: e.matmul(bankf(bk_)[:, hs * nq:(hs + 1) * nq], sbms[0:64, :], identb[0:64, 0:64],
                                                                       start=False, stop=False, skip_group_check=True),
                                         reads=[Bc], writes=[Bbank[bk_]])

                    def st0(kb, u):
                        zmm(0 + (u % 2), kb, kb >= NKB - ND, kb - (NKB - ND))

                    def st1(kb, u):
                        ba = 0 + (u % 2); bb = 2 + (u % 2)
                        RE = Re[u % 2]; ev = RE.view(F32, [512])
                        RSP = Rsp[u % 2]; spv = RSP.view(BF16, [512])
                        S.op("act", lambda e: e.activation(out=ev[:, 0:n], in_=bankf(ba)[:, 0:n], func=AF.Exp), reads=[Bbank[ba]], writes=[RE.b])
                        S.op("act", lambda e: e.activation(out=spv[:, 0:n], in_=ev[:, 0:n], func=AF.Ln, bias=onef[:, 0:1], scale=1.0), reads=[RE.b, Bonef], writes=[RSP.b])
                        zmm(bb, kb, kb >= NKB - ND, kb - (NKB - ND))
                        last = (u == 0)
                        S.op("pe", lambda e: e.matmul(bankf(bb)[:, 0:n], lneg[:, :], spv[:, 0:n], start=False, stop=last, skip_group_check=True), reads=[Bc, RSP.b], writes=[Bbank[bb]])
                        if u > 0:
                            S.op("pe", lambda e: e.matmul(bankf(bb)[:, 0:n], oneg[:, :], Sl[:, 0:n], start=False, stop=True, skip_group_check=True), reads=[Bc, RS.b], writes=[Bbank[bb]])
                        if u < NKB - 1:
                            S.op("pool", lambda e: e.tensor_tensor(out=Sl[:, 0:n], in0=Sl[:, 0:n], in1=spv[:, 0:n], op=ALU.add), reads=[RS.b, RSP.b], writes=[RS.b])

                    def st2(kb, u):
                        bb = 2 + (u % 2)
                        RP = RPt[u % 2]; Pt = RP.view(BF16, [512])
                        RSP = Rsp[u % 2]; spv = RSP.view(BF16, [512])
                        S.op("act", lambda e: e.activation(out=Pt[:, 0:n], in_=bankf(bb)[:, 0:n], func=AF.Exp), reads=[Bbank[bb]], writes=[RP.b])
                        for hs in range(HG):
                            h = g * HG + hs
                            S.op("pe", lambda e, hs=hs, h=h: e.matmul(bankf(bo)[64:128, hs * nq:(hs + 1) * nq], vb[:, kb, h * 64:(h + 1) * 64], Pt[:, hs * nq:(hs + 1) * nq],
                                                                    start=(u == 0 and hs == 0), stop=(u == NKB - 1), skip_group_check=True),
                                 reads=[Rvb.b, RP.b], writes=[Bbank[bo]])

                    for u in range(NKB + 2):
                        if u < NKB:
                            st0(order[u], u)
                        if 1 <= u <= NKB:
                            st1(order[u - 1], u - 1)
                        if u >= 2:
                            st2(order[u - 2], u - 2)
                    S.op("dve", lambda e, g=g: e.tensor_copy(out=oT[64:128, g * HG:(g + 1) * HG, tok0:tok0 + nq],
                                                            in_=bankf(bo)[64:128, 0:n].rearrange("p (a b) -> p a b", a=HG)),
                         reads=[Bbank[bo]], writes=[RoT.buf("b")])

            qside(0)
            for j in range(nblk):
                if j + 1 < nblk:
                    qside(j + 1)
                attn(j)
            for r in [Rqs] + RqbT + Re + Rsp + RPt + RSl + [RkbT, Rvb]:
                AR.release(r)

        def early():
            S.finish()
            build.info = dict(peak=AR.peak, nsem=S.nsem, cnt=dict(S.cnt))
            return nc, es
        if upto <= 0:
            return early()
        mixer_A("p")
        if upto <= 1:
            return early()
        mixer_A("s")
        if upto <= 2:
            return early()
        mixer_B("p")
        if upto <= 3:
            return early()
        mixer_B("s")
        if upto <= 4:
            return early()

        phase0(True)
        RG1 = GR["RG1"]; RG2 = GR["RG2"]
        G1 = RG1.view(F32, [2, 1024]); G2 = RG2.view(F32, [2, 1024])
        NTOK = TOWN
        tiles = [(m, 128, m * 128, 0) for m in range(NOWN)] + [(NOWN, 64, NOWN * 128, 1)]
        Rh1 = A(8 * NTOK * 2, "h1T"); h1T = Rh1.view(BF16, [8, NTOK])
        for (m, rows, tok0, j) in tiles:
            Rx = Rxs[m % 2]; xs = Rx.view(F32, [1024])
            src = x_own[m * 128:(m + 1) * 128, :] if j == 0 else x_s[:, :]
            S.dma("sp", xs[:rows, :], src, wbuf=Rx.b)
            norm_tile(xs[:rows, :], Rx.b, rows, A1v, B1v, j, h1T[:, :, tok0:tok0 + rows], Rh1, 0, 1)
        Rmg = A(8 * NTOK * 2, "mergedT"); mergedT = Rmg.view(BF16, [8, NTOK])
        Rgs = [A(8 * 256 * 2, f"gslab{i}") for i in range(2)]
        Rwu = [A(8 * 128 * 2, f"wu{i}") for i in range(2)]
        Rga = [A(512 * 4, f"ga{i}") for i in range(2)]
        Rgb = [A(512 * 4, f"gb{i}") for i in range(2)]
        Rm1 = [A(512 * 4, f"m1{i}") for i in range(2)]
        Rm2 = [A(512 * 4, f"m2{i}") for i in range(2)]
        chunks = [(c * 512, 512) for c in range(4)] + [(2048, 64)]

        def mgb(t0, n):
            return [Rmg.buf(m) for m in range(t0 // 128, (t0 + n + 127) // 128)]
        ci = 0
        for i in range(8):
            Rg = Rgs[i % 2]; gsl = Rg.view(BF16, [8, 256])
            S.dma("pool", gsl[:, :, 0:128], w_in_v[:, :, C_G + i * 128:C_G + (i + 1) * 128], wbuf=Rg.b)
            S.dma("pool", gsl[:, :, 128:256], w_in_v[:, :, C_G + 1024 + i * 128:C_G + 1024 + (i + 1) * 128], wbuf=Rg.b, more=True)
            Rw = Rwu[i % 2]; wu = Rw.view(BF16, [8, 128])
            S.dma("pool", wu, w_up[:, :, i * 128:(i + 1) * 128], wbuf=Rw.b)
            for (t0, n) in chunks:
                pb = (ci % 2) * 4
                ga = Rga[ci % 2].view(F32, [512]); gb = Rgb[ci % 2].view(F32, [512])
                m1 = Rm1[ci % 2].view(F32, [512]); m2 = Rm2[ci % 2].view(F32, [512])
                for k in range(8):
                    S.op("pe", lambda e, k=k: e.matmul(bankf(pb)[:, 0:n], gsl[:, k, 0:128], h1T[:, k, t0:t0 + n], start=(k == 0), stop=(k == 7)), reads=[Rg.b, Rh1.buf(k)], writes=[Bbank[pb]])
                for k in range(8):
                    S.op("pe", lambda e, k=k: e.matmul(bankf(pb + 1)[:, 0:n], gsl[:, k, 128:256], h1T[:, k, t0:t0 + n], start=(k == 0), stop=(k == 7)), reads=[Rg.b, Rh1.buf(k)], writes=[Bbank[pb + 1]])
                for h in range(8):
                    S.op("pe", lambda e, h=h: e.matmul(bankf(pb + 2)[:, 0:n], wu[0:64, h, :], oT[0:64, h, t0:t0 + n], start=(h == 0), stop=(h == 7)), reads=[Rw.b, RoT.buf("a")], writes=[Bbank[pb + 2]])
                for h in range(8):
                    S.op("pe", lambda e, h=h: e.matmul(bankf(pb + 3)[:, 0:n], wu[64:128, h, :], oT[64:128, h, t0:t0 + n], start=(h == 0), stop=(h == 7)), reads=[Rw.b, RoT.buf("b")], writes=[Bbank[pb + 3]])
                S.op("act", lambda e: e.activation(out=ga[:, 0:n], in_=bankf(pb)[:, 0:n], func=AF.Sigmoid), reads=[Bbank[pb]], writes=[Rga[ci % 2].b])
                S.op("act", lambda e: e.activation(out=gb[:, 0:n], in_=bankf(pb + 1)[:, 0:n], func=AF.Sigmoid), reads=[Bbank[pb + 1]], writes=[Rgb[ci % 2].b])
                S.op("dve", lambda e: e.tensor_tensor(out=m1[:, 0:n], in0=bankf(pb + 2)[:, 0:n], in1=ga[:, 0:n], op=ALU.mult), reads=[Bbank[pb + 2], Rga[ci % 2].b], writes=[Rm1[ci % 2].b])
                S.op("dve", lambda e: e.tensor_tensor(out=m2[:, 0:n], in0=bankf(pb + 3)[:, 0:n], in1=gb[:, 0:n], op=ALU.mult), reads=[Bbank[pb + 3], Rgb[ci % 2].b], writes=[Rm2[ci % 2].b])
                S.op("pool", lambda e, i=i: e.tensor_tensor(out=mergedT[:, i, t0:t0 + n], in0=m1[:, 0:n], in1=m2[:, 0:n], op=ALU.add), reads=[Rm1[ci % 2].b, Rm2[ci % 2].b], writes=mgb(t0, n))
                ci += 1
        for r in Rgs + Rwu + Rga + Rgb + Rm1 + Rm2 + [Rh1, RoT]:
            AR.release(r)

        if upto <= 5:
            return early()
        for r in [Rxn] + RhT + list(Rst.values()) + [Rbf, Rbf2, Rt1]:
            AR.release(r)
        Rwo = A(8 * 1024 * 2, "wout"); wo = Rwo.view(BF16, [8, 1024])
        S.dma("pool", wo, w_out_v, wbuf=Rwo.b)
        Ry = [A(1024 * 4, f"y{m}") for m in range(17)]
        yv = [r.view(F32, [1024]) for r in Ry]
        h2T = mergedT
        Rh2fs = [A(8 * 128 * 4, f"h2Tf{i}") for i in range(2)]
        Rcomb = A(17 * 32 * 4, "comb"); comb = Rcomb.view(F32, [17, 32])
        Rtys = [A(1024 * 4, f"ty{i}") for i in range(2)]
        Rxnfs = [A(1024 * 4, f"xnf{i}") for i in range(2)]
        NTL = 17
        Rrs = A(NTL * 160 * 4, "route")
        _ro = [0]

        def rsl(n):
            v = Rrs.view(F32, [NTL, n], _ro[0]) if n > 1 else Rrs.view(F32, [NTL], _ro[0])
            _ro[0] += NTL * n * 4
            return v
        lgall = rsl(36); gmxa = rsl(1); goha = rsl(4); dga = rsl(4); gexa = rsl(4); gsuma = rsl(1); pga = rsl(1)
        eta = rsl(32); esela = rsl(8); m1a = rsl(1); oh1a = rsl(8); e2a = rsl(8); m2a = rsl(1); oh2a = rsl(8)
        dda = rsl(1); ex2a = rsl(1); w1a = rsl(1); w2a = rsl(1); c8a = rsl(8)
        Brt = Rrs.b
        S.op("pool", lambda e: e.memset(lgall, 0.0), writes=[Brt])
        for (m, rows, tok0, j) in tiles:
            yb = 0 if m % 2 == 0 else 2
            Rh2f = Rh2fs[m % 2]; h2f = Rh2f.view(F32, [8, 128])
            Rty = Rtys[m % 2]; ty = Rty.view(F32, [1024])
            XF["v"] = Rxnfs[m % 2].view(F32, [1024]); XF["b"] = Rxnfs[m % 2].b
            for half in range(2):
                for k in range(8):
                    S.op("pe", lambda e, k=k, half=half: e.matmul(bankf(yb + half)[:rows, :], mergedT[:, k, tok0:tok0 + rows], wo[:, k, half * 512:(half + 1) * 512], start=(k == 0), stop=(k == 7)),
                         reads=[Rmg.buf(m), Rwo.b], writes=[Bbank[yb + half]])
            Rx = Rxs[m % 2]; xs = Rx.view(F32, [1024])
            src = x_own[m * 128:(m + 1) * 128, :] if j == 0 else x_s[:, :]
            S.dma("sp", xs[:rows, :], src, wbuf=Rx.b)
            Bym = Ry[m].b
            for half in range(2):
                S.op("dve", lambda e, half=half: e.tensor_tensor(out=ty[:rows, half * 512:(half + 1) * 512], in0=bankf(yb + half)[:rows, :], in1=G1[:rows, j, half * 512:(half + 1) * 512], op=ALU.mult),
                     reads=[Bbank[yb + half], RG1.b], writes=[Rty.b])
            S.op("pool", lambda e, m=m: e.tensor_tensor(out=yv[m][:rows, :], in0=ty[:rows, :], in1=xs[:rows, :], op=ALU.add), reads=[Rty.b, Rx.b], writes=[Bym])
            tb1 = 4 if m % 2 == 0 else 6
            norm_tile(yv[m][:rows, :], Bym, rows, A2v, B2v, j, h2f, Rh2f, tb1, tb1 + 1, fp32=True)
            S.op("pool", lambda e: e.tensor_copy(out=h2T[:, :, tok0:tok0 + rows], in_=h2f[:, :, :rows]), reads=[Rh2f.buf(k_) for k_ in range(8)], writes=[Rmg.buf(m)])
            for k in range(8):
                S.op("pe", lambda e, k=k: e.matmul(bankf(tb1)[:rows, 0:36], h2f[:, k, :rows], wr_sb[:, k, :], start=(k == 0), stop=(k == 7)), reads=[Rh2f.buf(k), Bc2], writes=[Bbank[tb1]])
            S.op("dve", lambda e, m=m: e.tensor_tensor(out=lgall[:rows, m, :], in0=bankf(tb1)[:rows, 0:36], in1=brr[:rows, :], op=ALU.add), reads=[Bbank[tb1], Bc2], writes=[Brt])
        P_ = 128

        def dv(fn):
            S.op("dve", fn, reads=[Brt], writes=[Brt])
        lgg = lgall[:, :, 0:4]
        lge = lgall[:, :, 4:36].rearrange("p t (g x) -> p t g x", g=4)
        dv(lambda e: e.reduce_max(out=gmxa, in_=lgg, axis=AX.X))
        dv(lambda e: e.tensor_tensor(out=goha, in0=lgg, in1=gmxa.unsqueeze(2).to_broadcast([P_, NTL, 4]), op=ALU.is_ge))
        dv(lambda e: e.tensor_tensor(out=dga, in0=lgg, in1=gmxa.unsqueeze(2).to_broadcast([P_, NTL, 4]), op=ALU.subtract))
        S.op("act", lambda e: e.activation(out=gexa, in_=dga, func=AF.Exp), reads=[Brt], writes=[Brt])
        dv(lambda e: e.reduce_sum(out=gsuma, in_=gexa, axis=AX.X))
        dv(lambda e: e.reciprocal(out=pga, in_=gsuma))
        etv = eta.rearrange("p t (g x) -> p t g x", g=4)
        dv(lambda e: e.tensor_tensor(out=etv, in0=lge, in1=goha.unsqueeze(3).to_broadcast([P_, NTL, 4, 8]), op=ALU.mult))
        dv(lambda e: e.reduce_sum(out=esela, in_=eta.rearrange("p t (g x) -> p t x g", g=4), axis=AX.X))
        dv(lambda e: e.reduce_max(out=m1a, in_=esela, axis=AX.X))
        dv(lambda e: e.tensor_tensor(out=oh1a, in0=esela, in1=m1a.unsqueeze(2).to_broadcast([P_, NTL, 8]), op=ALU.is_ge))
        dv(lambda e: e.scalar_tensor_tensor(out=e2a, in0=oh1a, scalar=-1e30, in1=esela, op0=ALU.mult, op1=ALU.add))
        dv(lambda e: e.reduce_max(out=m2a, in_=e2a, axis=AX.X))
        dv(lambda e: e.tensor_tensor(out=oh2a, in0=e2a, in1=m2a.unsqueeze(2).to_broadcast([P_, NTL, 8]), op=ALU.is_ge))
        dv(lambda e: e.tensor_tensor(out=dda, in0=m2a, in1=m1a, op=ALU.subtract))
        S.op("act", lambda e: e.activation(out=ex2a, in_=dda, func=AF.Exp), reads=[Brt], writes=[Brt])
        dv(lambda e: e.tensor_scalar(out=w1a, in0=ex2a, scalar1=1.0, scalar2=None, op0=ALU.add))
        dv(lambda e: e.reciprocal(out=w1a, in_=w1a))
        dv(lambda e: e.tensor_tensor(out=w1a, in0=w1a, in1=pga, op=ALU.mult))
        dv(lambda e: e.tensor_tensor(out=w2a, in0=w1a, in1=ex2a, op=ALU.mult))
        dv(lambda e: e.tensor_tensor(out=oh1a, in0=oh1a, in1=w1a.unsqueeze(2).to_broadcast([P_, NTL, 8]), op=ALU.mult))
        dv(lambda e: e.tensor_tensor(out=oh2a, in0=oh2a, in1=w2a.unsqueeze(2).to_broadcast([P_, NTL, 8]), op=ALU.mult))
        dv(lambda e: e.tensor_tensor(out=c8a, in0=oh1a, in1=oh2a, op=ALU.add))
        S.op("dve", lambda e: e.tensor_tensor(out=comb.rearrange("p t (g x) -> p t g x", g=4), in0=goha.unsqueeze(3).to_broadcast([P_, NTL, 4, 8]),
                                             in1=c8a.unsqueeze(2).to_broadcast([P_, NTL, 4, 8]), op=ALU.mult), reads=[Brt], writes=[Rcomb.b])
        for r in [Rwo, Rrs] + Rh2fs + Rtys + Rxnfs + Rxs + [Rtmp, RG1]:
            AR.release(r)

        if upto <= 6:
            return early()
        Rgu = [A(8 * 512 * 2, f"gu{i}") for i in range(4)]
        Rdn = [A(2 * 1024 * 2, f"dn{i}") for i in range(4)]
        Rdp = [A(2 * 1024 * 2, f"dnp{i}") for i in range(4)]
        Rsg = [A(256 * 4, f"sg{i}") for i in range(2)]
        Rac = [A(256 * 2, f"ac{i}") for i in range(2)]
        RaT = [A(256 * 2, f"aT{i}") for i in range(2)]
        Rys = A(1024 * 4, "ys"); ysv = Rys.view(F32, [1024])
        G2b = G2[:, 0, :]
        NGRP = NEXP // 2
        loaded = set()

        def load_group(grp):
            if grp in loaded or grp >= NGRP:
                return
            loaded.add(grp)
            for ei in range(2):
                ex = grp * 2 + ei
                sl = (grp % 2) * 2 + ei
                gu = Rgu[sl].view(BF16, [8, 512])
                S.dma("pool", gu[:, :, 0:256], w_eg[ex].rearrange("(kc p) n -> p kc n", p=128), wbuf=Rgu[sl].b)
                S.dma("pool", gu[:, :, 256:512], w_eu[ex].rearrange("(kc p) n -> p kc n", p=128), wbuf=Rgu[sl].b, more=True)
                dn = Rdn[sl].view(BF16, [2, 1024])
                S.dma("pool", dn, w_ed[ex].rearrange("(fc p) n -> p fc n", p=128), wbuf=Rdn[sl].b)

        def scale_group(grp):
            for ei in range(2):
                sl = (grp % 2) * 2 + ei
                dn = Rdn[sl].view(BF16, [2, 1024]); dp = Rdp[sl].view(BF16, [2, 1024])
                S.op("dve", lambda e: e.tensor_tensor(out=dp, in0=dn, in1=G2b.unsqueeze(1).to_broadcast([128, 2, 1024]), op=ALU.mult), reads=[Rdn[sl].b, RG2.b], writes=[Rdp[sl].b])

        units = [(grp, m, rows, tok0, j, ei) for grp in range(NGRP) for (m, rows, tok0, j) in tiles for ei in range(2)]

        def stA(u):
            grp, m, rows, tok0, j, ei = units[u]
            sl = (grp % 2) * 2 + ei
            if m == 0 and ei == 0:
                load_group(grp)
                scale_group(grp)
            if m == 8 and ei == 0:
                load_group(grp + 1)
            gu = Rgu[sl].view(BF16, [8, 512])
            gb_ = u % 2
            for k in range(8):
                S.op("pe", lambda e, k=k: e.matmul(bankf(gb_)[:rows, :], h2T[:, k, tok0:tok0 + rows], gu[:, k, :], start=(k == 0), stop=(k == 7)), reads=[Rmg.buf(m), Rgu[sl].b], writes=[Bbank[gb_]])
            sg = Rsg[u % 2].view(F32, [256]); ac = Rac[u % 2].view(BF16, [256])
            ex = grp * 2 + ei
            S.op("act", lambda e: e.activation(out=sg[:rows, :], in_=bankf(gb_)[:rows, 0:256], func=AF.Silu), reads=[Bbank[gb_]], writes=[Rsg[u % 2].b])
            S.op("dve", lambda e: e.scalar_tensor_tensor(out=ac[:rows, :], in0=sg[:rows, :], scalar=comb[:rows, m, ex:ex + 1], in1=bankf(gb_)[:rows, 256:512], op0=ALU.mult, op1=ALU.mult),
                 reads=[Rsg[u % 2].b, Rcomb.b, Bbank[gb_]], writes=[Rac[u % 2].b])

        def stB(u):
            grp, m, rows, tok0, j, ei = units[u]
            ac = Rac[u % 2].view(BF16, [256]); aT = RaT[u % 2].view(BF16, [2, 128])
            tb = 2 + (u % 2)
            tpv = bankb(tb)
            for f in range(2):
                S.op("pe", lambda e, f=f: e.transpose(tpv[:, f * 128:f * 128 + rows], ac[:rows, f * 128:(f + 1) * 128], identb[:rows, :rows]), reads=[Rac[u % 2].b, Bc], writes=[Bbank[tb]])
            S.op("act", lambda e: e.activation(out=aT[:, :, :rows], in_=tpv[:, 0:256].rearrange("p (a b) -> p a b", a=2)[:, :, :rows], func=AF.Copy), reads=[Bbank[tb]], writes=[RaT[u % 2].b])

        def stC(u):
            grp, m, rows, tok0, j, ei = units[u]
            sl = (grp % 2) * 2 + ei
            aT = RaT[u % 2].view(BF16, [2, 128])
            yb = 4 if m % 2 == 0 else 6
            dnx = Rdp[sl].view(BF16, [2, 1024]) if j == 0 else Rdn[sl].view(BF16, [2, 1024])
            Bdnx = Rdp[sl].b if j == 0 else Rdn[sl].b
            for half in range(2):
                for f in range(2):
                    S.op("pe", lambda e, f=f, half=half: e.matmul(bankf(yb + half)[:rows, :], aT[:, f, :rows], dnx[:, f, half * 512:(half + 1) * 512],
                                                                start=(ei == 0 and f == 0), stop=(ei == 1 and f == 1), skip_group_check=True),
                         reads=[RaT[u % 2].b, Bdnx], writes=[Bbank[yb + half]])
            if ei == 1:
                Bym = Ry[m].b
                for half in range(2):
                    ysl = yv[m][:rows, half * 512:(half + 1) * 512]
                    if j == 0:
                        S.op("dve", lambda e, half=half, ysl=ysl: e.tensor_tensor(out=ysl, in0=bankf(yb + half)[:rows, :], in1=ysl, op=ALU.add), reads=[Bbank[yb + half], Bym], writes=[Bym])
                    else:
                        S.op("dve", lambda e, half=half: e.tensor_tensor(out=ysv[:rows, half * 512:(half + 1) * 512], in0=bankf(yb + half)[:rows, :], in1=G2[:rows, 1, half * 512:(half + 1) * 512], op=ALU.mult),
                             reads=[Bbank[yb + half], RG2.b], writes=[Rys.b])
                        S.op("pool", lambda e, half=half, ysl=ysl: e.tensor_tensor(out=ysl, in0=ysl, in1=ysv[:rows, half * 512:(half + 1) * 512], op=ALU.add), reads=[Rys.b, Bym], writes=[Bym])

        NU = len(units)
        load_group(0)
        for step in range(NU + 2):
            if step < NU:
                stA(step)
            if 1 <= step <= NU:
                stB(step - 1)
            if step >= 2:
                stC(step - 2)
        for (m, rows, tok0, j) in tiles:
            dst = y_own[m * 128:(m + 1) * 128, :] if j == 0 else y_s[:, :]
            S.dma("sp", dst, yv[m][:rows, :], rbuf=Ry[m].b, is_output=True)

    except Stop:
        pass
    S.finish()
    build.info = dict(peak=AR.peak, nsem=S.nsem, cnt=dict(S.cnt))
    return nc, es


def _consts(par):
    bf = ml_dtypes.bfloat16
    c = {}
    c["c_identb"] = np.eye(128, dtype=np.float32).astype(bf)
    c["c_identf"] = np.eye(128, dtype=np.float32)
    jj, ss = np.meshgrid(np.arange(128), np.arange(128), indexing="ij")
    c["c_lneg"] = np.where(jj >= ss, -1.0, 0.0).astype(np.float32).astype(bf)
    c["c_oneg"] = np.full((128, 128), -1.0, np.float32).astype(bf)
    T = TPAR[par]
    sbm = np.zeros((128, 4, 2, 128), np.float32)
    tq, sk = np.meshgrid(np.arange(128), np.arange(128), indexing="ij")
    for b in range(4):
        for w in range(2):
            r = T[w]
            if b < r:
                vis = np.ones((128, 128), bool)
            elif b == r:
                vis = sk < tq
            else:
                vis = np.zeros((128, 128), bool)
            sbm[:, b, w, :] = np.where(vis, 0.0, NEG)
    c["c_sbm"] = sbm.reshape(128, -1).astype(bf)
    tq, sk = np.meshgrid(np.arange(64), np.arange(128), indexing="ij")
    c["c_sbms"] = np.where(sk < tq, 0.0, NEG).astype(np.float32).astype(bf)
    adm = np.zeros((128, 2, 512), np.float32)
    t = np.arange(128)[:, None]
    s = np.arange(512)[None, :]
    for w in range(2):
        r = T[w]
        ok = (s // 64) <= (2 * r + t // 64)
        adm[:, w, :] = np.where(ok, 0.0, -1e30)
    c["c_adm"] = adm.reshape(128, -1)
    freqs = (np.float32(500000.0) ** (-np.arange(0, 16, 2, dtype=np.float32) / np.float32(16))).astype(np.float32)

    def rope_tab(pos):
        ang = pos.astype(np.float32)[..., None] * freqs[None, None, :]
        cs = np.cos(ang).astype(np.float32)
        sn = np.sin(ang).astype(np.float32)
        return np.concatenate([cs, cs, sn], axis=-1).astype(np.float32)
    p = np.arange(128)[:, None]
    c["c_ropep"] = rope_tab(np.arange(NTP)[None, :] * 128 + p).reshape(128, -1)
    own_g = np.array([4 * (m // 2) + T[m % 2] for m in range(NOWN)])
    c["c_ropeo"] = rope_tab(own_g[None, :] * 128 + p).reshape(128, -1)
    c["c_ropes"] = rope_tab(4096 + p).reshape(128, -1)
    c["c_pow2"] = np.tile((2.0 ** -np.arange(24, dtype=np.float64)).astype(np.float32)[None, :], (128, 1))
    return c, own_g


_CACHE = {}


def kernel(x_prompt, x_sample, cache_a_k, cache_a_v, cache_a_kidx, cache_b_k, cache_b_v, c_prompt, c_sample,
           w_ada, b_ada, norm1_g, w_in, qnorm_g, knorm_g, w_up_a, w_up_b, w_out, norm2_g,
           w_rg, b_rg, w_re, b_re, w_e_gate, w_e_up, w_e_down):
    f = lambda a: np.ascontiguousarray(np.asarray(a, dtype=np.float32))
    x_prompt = f(x_prompt); x_sample = f(x_sample)
    if "nc" not in _CACHE:
        import os
        _CACHE["nc"] = build(float(os.environ.get("K_UPTO", "9")))
    nc, _es = _CACHE["nc"]
    shared = {
        "w_ada": f(w_ada[0]), "b_adaT": f(np.asarray(b_ada[0]).reshape(48, 128).T), "b_ada": f(np.asarray(b_ada[0]).reshape(1, -1)),
        "n1gT": f(np.asarray(norm1_g[0]).reshape(8, 128).T), "n2gT": f(np.asarray(norm2_g[0]).reshape(8, 128).T),
        "gq": f(np.asarray(qnorm_g[0]).reshape(1, 64)), "gk": f(np.asarray(knorm_g[0]).reshape(1, 64)),
        "w_in": f(w_in[0]),
        "w_up": f(np.concatenate([np.asarray(w_up_a[0]).reshape(8, 64, D).transpose(1, 0, 2), np.asarray(w_up_b[0]).reshape(8, 64, D).transpose(1, 0, 2)], axis=0)),
        "w_out": f(w_out[0]),
        "w_r": f(np.concatenate([np.asarray(w_rg[0]), np.asarray(w_re[0])], axis=1)),
        "b_r": f(np.concatenate([np.asarray(b_rg[0]), np.asarray(b_re[0])]).reshape(1, 36)),
        "w_eg": f(w_e_gate[0]), "w_eu": f(w_e_up[0]), "w_ed": f(w_e_down[0]),
    }
    in_maps = []
    owns = []
    for c in range(8):
        b, par = c // 2, c % 2
        cst, own_g = _consts(par)
        owns.append(own_g)
        rows = np.concatenate([np.arange(g * 128, (g + 1) * 128) for g in own_g])
        m = dict(shared)
        m.update(cst)
        m["x_p"] = x_prompt[b]
        m["x_own"] = np.ascontiguousarray(x_prompt[b][rows])
        m["x_s"] = x_sample[c]
        m["ca_k"] = f(np.asarray(cache_a_k[0, c]).reshape(4096, 512)); m["ca_v"] = f(np.asarray(cache_a_v[0, c]).reshape(4096, 512))
        m["ca_ki"] = f(cache_a_kidx[0, c])
        m["cb_k"] = f(np.asarray(cache_b_k[0, c]).reshape(4096, 512)); m["cb_v"] = f(np.asarray(cache_b_v[0, c]).reshape(4096, 512))
        cc_ = np.stack([np.asarray(c_prompt[b]), np.asarray(c_sample[c])], axis=1)
        m["cT"] = f(cc_.reshape(8, 128, 2).transpose(1, 0, 2).reshape(128, 16))
        in_maps.append(m)
    import os
    ncore = int(os.environ.get("K_CORES", "8"))
    res = run_bass_kernel_spmd(nc, in_maps[:ncore], core_ids=list(range(ncore)))
    R = list(res.results) + [res.results[0]] * (8 - ncore)
    y_prompt = np.zeros((4, 4096, D), np.float32)
    y_sample = np.zeros((8, 64, D), np.float32)
    outs_p = {k: np.zeros((1, 4) + s, np.float32) for k, s in
              (("o_nak", (4096, 8, 64)), ("o_nav", (4096, 8, 64)), ("o_naki", (4096, 64)), ("o_nbk", (4096, 8, 64)), ("o_nbv", (4096, 8, 64)))}
    outs_s = {k: np.zeros((1, 8) + s, np.float32) for k, s in
              (("s_nak", (64, 8, 64)), ("s_nav", (64, 8, 64)), ("s_naki", (64, 64)), ("s_nbk", (64, 8, 64)), ("s_nbv", (64, 8, 64)))}
    for c in range(8):
        b = c // 2
        yo = R[c]["y_own"]
        for mi, g in enumerate(owns[c]):
            y_prompt[b, g * 128:(g + 1) * 128] = yo[mi * 128:(mi + 1) * 128]
        y_sample[c] = R[c]["y_s"]
        if c % 2 == 0:
            for k in outs_p:
                outs_p[k][0, b] = R[c][k].reshape(outs_p[k].shape[2:])
        for k in outs_s:
            outs_s[k][0, c] = R[c][k].reshape(outs_s[k].shape[2:])
    return (y_prompt, y_sample, outs_p["o_nak"], outs_p["o_nav"], outs_p["o_naki"], outs_p["o_nbk"], outs_p["o_nbv"],
            outs_s["s_nak"], outs_s["s_nav"], outs_s["s_naki"], outs_s["s_nbk"], outs_s["s_nbv"])
```
